# Optimizing a Trainium2 kernel written in Bass

```python
import jax
import jax.numpy as jnp
from jax import lax
import numpy as np

D_MODEL = 1024
BATCH = 4
SEQ = 4096
DEPTH = 2
DEC_BATCH = 4
DEC_SEQ = 8192
PAST_LEN = 128

GRID_W = 64
ROPE_THETA = 10000.0
Q_BLOCK = 128
EPS = 1e-6
N_MEM = 256
MEM_HEADS = 4
MEM_HD = D_MODEL // MEM_HEADS
GQA_HEADS = 8
GQA_KV_HEADS = 2
GQA_HD = 128
MLA_HEADS = 8
MLA_Q_LORA = 384
MLA_KV_LORA = 256
MLA_NOPE = 64
MLA_ROPE = 32
MLA_V = 128
N_BRANCH = 3
N_GROUPS = 8
EXPERTS_PER_GROUP = 8
N_EXPERTS = N_GROUPS * EXPERTS_PER_GROUP
TOP_K = 2
D_EXPERT = 256
MOE_BLOCK = 128

GQA_Q_W = GQA_HEADS * GQA_HD
GQA_KV_W = GQA_KV_HEADS * GQA_HD
MEM_Q_W = MEM_HEADS * MEM_HD
GATE_W = N_BRANCH * D_MODEL
SPLIT_1 = GQA_Q_W
SPLIT_2 = SPLIT_1 + GQA_KV_W
SPLIT_3 = SPLIT_2 + GQA_KV_W
SPLIT_4 = SPLIT_3 + MLA_Q_LORA
SPLIT_5 = SPLIT_4 + MLA_KV_LORA
SPLIT_6 = SPLIT_5 + MLA_ROPE
SPLIT_7 = SPLIT_6 + MEM_Q_W
IN_W = SPLIT_7 + GATE_W
IN_SPLITS = (SPLIT_1, SPLIT_2, SPLIT_3, SPLIT_4, SPLIT_5, SPLIT_6, SPLIT_7)

kernel_name = "hybrid_gqa_mla_memxattn_hmoe_encoder"


def rmsnorm(x, g):
    xf = x.astype(jnp.float32)
    y = xf * lax.rsqrt(jnp.mean(xf * xf, axis=-1, keepdims=True) + EPS)
    return (y * g.astype(jnp.float32)).astype(x.dtype)


def axial_rope_tables(n_tokens, rot_dim):
    rows = n_tokens // GRID_W
    row_idx = jnp.repeat(jnp.arange(rows, dtype=jnp.float32), GRID_W)
    col_idx = jnp.tile(jnp.arange(GRID_W, dtype=jnp.float32), rows)
    n_freq = rot_dim // 4
    inv_freq = ROPE_THETA ** (-jnp.arange(n_freq, dtype=jnp.float32) / n_freq)
    ang = jnp.concatenate([row_idx[:, None] * inv_freq, col_idx[:, None] * inv_freq], axis=-1)
    return jnp.cos(ang), jnp.sin(ang)


def apply_rope(x, cos, sin):
    half = x.shape[-1] // 2
    xf = x.astype(jnp.float32)
    x1, x2 = xf[..., :half], xf[..., half:]
    c = cos[None, :, None, :]
    s = sin[None, :, None, :]
    return jnp.concatenate([x1 * c - x2 * s, x2 * c + x1 * s], axis=-1).astype(x.dtype)


def block_attention(q, k, v, scale):
    b, s, h, d = q.shape
    hkv = k.shape[2]
    g = h // hkv
    nblk = s // Q_BLOCK
    qb = q.reshape(b, nblk, Q_BLOCK, hkv, g, d).transpose(1, 0, 2, 3, 4, 5)

    def one_block(qi):
        sc = jnp.einsum('bqkgd,btkd->bkgqt', qi, k, preferred_element_type=jnp.float32) * scale
        p = jax.nn.softmax(sc, axis=-1).astype(v.dtype)
        return jnp.einsum('bkgqt,btke->bqkge', p, v)

    out = lax.map(one_block, qb)
    return out.transpose(1, 0, 2, 3, 4, 5).reshape(b, s, h, v.shape[-1])


def mixer_sublayer(h, mem, cos_g, sin_g, cos_m, sin_m, norm_mem, w_in, gqa_q_norm, gqa_k_norm,
                   mla_q_a_norm, mla_kv_a_norm, mla_w_qb, mla_w_kvb, mem_w_kv, w_out):
    b, s, _ = h.shape
    proj = h @ w_in
    q_g, k_g, v_g, q_a, kv_a, k_r, q_c, gates = jnp.split(proj, IN_SPLITS, axis=-1)

    q_g = apply_rope(rmsnorm(q_g.reshape(b, s, GQA_HEADS, GQA_HD), gqa_q_norm), cos_g, sin_g)
    k_g = apply_rope(rmsnorm(k_g.reshape(b, s, GQA_KV_HEADS, GQA_HD), gqa_k_norm), cos_g, sin_g)
    v_g = v_g.reshape(b, s, GQA_KV_HEADS, GQA_HD)
    o_a = block_attention(q_g, k_g, v_g, GQA_HD ** -0.5).reshape(b, s, D_MODEL)

    q = (rmsnorm(q_a, mla_q_a_norm) @ mla_w_qb).reshape(b, s, MLA_HEADS, MLA_NOPE + MLA_ROPE)
    q_rope = apply_rope(q[..., MLA_NOPE:], cos_m, sin_m)
    kv = (rmsnorm(kv_a, mla_kv_a_norm) @ mla_w_kvb).reshape(b, s, MLA_HEADS, MLA_NOPE + MLA_V)
    k_nope, v_m = kv[..., :MLA_NOPE], kv[..., MLA_NOPE:]
    k_rope = apply_rope(k_r[:, :, None, :], cos_m, sin_m)
    q_full = jnp.concatenate([q[..., :MLA_NOPE], q_rope], axis=-1)
    k_full = jnp.concatenate([k_nope, jnp.broadcast_to(k_rope, (b, s, MLA_HEADS, MLA_ROPE))], axis=-1)
    o_b = block_attention(q_full, k_full, v_m, (MLA_NOPE + MLA_ROPE) ** -0.5).reshape(b, s, D_MODEL)

    mkv = rmsnorm(mem, norm_mem) @ mem_w_kv
    mk, mv = jnp.split(mkv, 2, axis=-1)
    n_mem = mem.shape[1]
    mk = mk.reshape(b, n_mem, MEM_HEADS, MEM_HD)
    mv = mv.reshape(b, n_mem, MEM_HEADS, MEM_HD)
    o_c = block_attention(q_c.reshape(b, s, MEM_HEADS, MEM_HD), mk, mv, MEM_HD ** -0.5).reshape(b, s, D_MODEL)

    gt = jax.nn.sigmoid(gates.reshape(b, s, N_BRANCH, D_MODEL).astype(jnp.float32)).astype(h.dtype)
    merged = gt[:, :, 0] * o_a + gt[:, :, 1] * o_b + gt[:, :, 2] * o_c
    return merged @ w_out


def hier_moe(h, w_group, b_group, w_expert, b_expert, w_gate_up, w_down):
    b, s, d = h.shape
    hf = h.reshape(-1, d)
    n = hf.shape[0]
    g_logits = (hf @ w_group).astype(jnp.float32) + b_group.astype(jnp.float32)
    g_prob = jax.nn.softmax(g_logits, axis=-1)
    g_idx = jnp.argmax(g_logits, axis=-1).astype(jnp.int32)
    g_w = jnp.take_along_axis(g_prob, g_idx[:, None], axis=-1)
    e_logits = ((hf @ w_expert).astype(jnp.float32) + b_expert.astype(jnp.float32)).reshape(n, N_GROUPS, EXPERTS_PER_GROUP)
    e_logits = jnp.take_along_axis(e_logits, g_idx[:, None, None], axis=1)[:, 0]
    top_val, top_loc = lax.top_k(e_logits, TOP_K)
    e_w = jax.nn.softmax(top_val, axis=-1) * g_w
    e_idx = g_idx[:, None] * EXPERTS_PER_GROUP + top_loc.astype(jnp.int32)

    m = n * TOP_K
    flat_e = e_idx.reshape(-1)
    order = jnp.argsort(flat_e)
    sorted_e = flat_e[order]
    tok = (order // TOP_K).astype(jnp.int32)
    w_sorted = e_w.reshape(-1)[order]
    sizes = jnp.bincount(flat_e, length=N_EXPERTS).astype(jnp.int32)
    starts = jnp.cumsum(sizes) - sizes
    pad_sizes = (sizes + MOE_BLOCK - 1) // MOE_BLOCK * MOE_BLOCK
    pad_ends = jnp.cumsum(pad_sizes)
    pad_starts = pad_ends - pad_sizes
    dest = pad_starts[sorted_e] + (jnp.arange(m, dtype=jnp.int32) - starts[sorted_e])
    n_blk = (m + MOE_BLOCK - 1) // MOE_BLOCK + N_EXPERTS
    p_len = n_blk * MOE_BLOCK
    buf_tok = jnp.zeros((p_len,), jnp.int32).at[dest].set(tok)
    buf_w = jnp.zeros((p_len,), jnp.float32).at[dest].set(w_sorted)
    blk_start = jnp.arange(n_blk, dtype=jnp.int32) * MOE_BLOCK
    blk_e = jnp.minimum(jnp.searchsorted(pad_ends, blk_start, side='right'), N_EXPERTS - 1).astype(jnp.int32)

    def one_block(args):
        tok_b, w_b, e = args
        xb = hf[tok_b]
        gu = xb @ w_gate_up[e]
        a, u = jnp.split(gu, 2, axis=-1)
        yb = (jax.nn.silu(a) * u) @ w_down[e]
        return yb * w_b[:, None].astype(yb.dtype)

    ys = lax.map(one_block, (buf_tok.reshape(n_blk, MOE_BLOCK), buf_w.reshape(n_blk, MOE_BLOCK), blk_e))
    y = jnp.zeros_like(hf).at[buf_tok].add(ys.reshape(p_len, d))
    return y.reshape(b, s, d)


def trunk(x, mem, norm_mix, norm_mem, w_in, gqa_q_norm, gqa_k_norm, mla_q_a_norm, mla_kv_a_norm,
          mla_w_qb, mla_w_kvb, mem_w_kv, w_out, norm_ffn, w_group, b_group, w_expert, b_expert,
          w_gate_up, w_down, norm_final):
    n_tok = x.shape[1]
    cos_g, sin_g = axial_rope_tables(n_tok, GQA_HD)
    cos_m, sin_m = axial_rope_tables(n_tok, MLA_ROPE)
    for l in range(DEPTH):
        x = x + mixer_sublayer(rmsnorm(x, norm_mix[l]), mem, cos_g, sin_g, cos_m, sin_m, norm_mem[l], w_in[l],
                               gqa_q_norm[l], gqa_k_norm[l], mla_q_a_norm[l], mla_kv_a_norm[l],
                               mla_w_qb[l], mla_w_kvb[l], mem_w_kv[l], w_out[l])
        x = x + hier_moe(rmsnorm(x, norm_ffn[l]), w_group[l], b_group[l], w_expert[l], b_expert[l],
                         w_gate_up[l], w_down[l])
    return rmsnorm(x, norm_final)


def setup_inputs(seed: int = 0) -> dict:
    key = jax.random.key(seed)
    ks = jax.random.split(key, 24)
    f32 = jnp.float32

    def nrm(k, shape, scale):
        return jax.random.normal(k, shape, f32) * scale

    def gain(k, shape):
        return 1.0 + 0.02 * jax.random.normal(k, shape, f32)

    return {
        'x_prompt': nrm(ks[0], (BATCH, SEQ, D_MODEL), 1.0),
        'x_sample': nrm(ks[1], (DEC_BATCH, DEC_SEQ, D_MODEL), 1.0),
        'mem_prompt': nrm(ks[2], (BATCH, N_MEM, D_MODEL), 1.0),
        'mem_sample': nrm(ks[3], (DEC_BATCH, N_MEM, D_MODEL), 1.0),
        'norm_mix': gain(ks[4], (DEPTH, D_MODEL)),
        'norm_mem': gain(ks[5], (DEPTH, D_MODEL)),
        'w_in': nrm(ks[6], (DEPTH, D_MODEL, IN_W), D_MODEL ** -0.5),
        'gqa_q_norm': gain(ks[7], (DEPTH, GQA_HD)),
        'gqa_k_norm': gain(ks[8], (DEPTH, GQA_HD)),
        'mla_q_a_norm': gain(ks[9], (DEPTH, MLA_Q_LORA)),
        'mla_kv_a_norm': gain(ks[10], (DEPTH, MLA_KV_LORA)),
        'mla_w_qb': nrm(ks[11], (DEPTH, MLA_Q_LORA, MLA_HEADS * (MLA_NOPE + MLA_ROPE)), MLA_Q_LORA ** -0.5),
        'mla_w_kvb': nrm(ks[12], (DEPTH, MLA_KV_LORA, MLA_HEADS * (MLA_NOPE + MLA_V)), MLA_KV_LORA ** -0.5),
        'mem_w_kv': nrm(ks[13], (DEPTH, D_MODEL, 2 * MEM_Q_W), D_MODEL ** -0.5),
        'w_out': nrm(ks[14], (DEPTH, D_MODEL, D_MODEL), D_MODEL ** -0.5),
        'norm_ffn': gain(ks[15], (DEPTH, D_MODEL)),
        'w_group': nrm(ks[16], (DEPTH, D_MODEL, N_GROUPS), D_MODEL ** -0.5),
        'b_group': nrm(ks[17], (DEPTH, N_GROUPS), 0.01),
        'w_expert': nrm(ks[18], (DEPTH, D_MODEL, N_EXPERTS), D_MODEL ** -0.5),
        'b_expert': nrm(ks[19], (DEPTH, N_EXPERTS), 0.01),
        'w_gate_up': nrm(ks[20], (DEPTH, N_EXPERTS, D_MODEL, 2 * D_EXPERT), D_MODEL ** -0.5),
        'w_down': nrm(ks[21], (DEPTH, N_EXPERTS, D_EXPERT, D_MODEL), D_EXPERT ** -0.5),
        'norm_final': gain(ks[22], (D_MODEL,)),
    }


def reference(x_prompt, x_sample, mem_prompt, mem_sample, norm_mix, norm_mem, w_in, gqa_q_norm, gqa_k_norm,
              mla_q_a_norm, mla_kv_a_norm, mla_w_qb, mla_w_kvb, mem_w_kv, w_out, norm_ffn, w_group, b_group,
              w_expert, b_expert, w_gate_up, w_down, norm_final):
    y_prompt = trunk(x_prompt, mem_prompt, norm_mix, norm_mem, w_in, gqa_q_norm, gqa_k_norm, mla_q_a_norm,
                     mla_kv_a_norm, mla_w_qb, mla_w_kvb, mem_w_kv, w_out, norm_ffn, w_group, b_group,
                     w_expert, b_expert, w_gate_up, w_down, norm_final)
    y_sample = trunk(x_sample, mem_sample, norm_mix, norm_mem, w_in, gqa_q_norm, gqa_k_norm, mla_q_a_norm,
                     mla_kv_a_norm, mla_w_qb, mla_w_kvb, mem_w_kv, w_out, norm_ffn, w_group, b_group,
                     w_expert, b_expert, w_gate_up, w_down, norm_final)
    return (y_prompt, y_sample)
```

```python
import contextlib
import numpy as np
import ml_dtypes
import concourse.bass as bass
import concourse.mybir as mybir
from concourse.bass_utils import run_bass_kernel_spmd

F32 = mybir.dt.float32
BF16 = mybir.dt.bfloat16
I32 = mybir.dt.int32
AF = mybir.ActivationFunctionType
ALU = mybir.AluOpType
AX = mybir.AxisListType

D = 1024
IN_W = 6304
C_QG, C_KG, C_VG, C_QA, C_KVA, C_KR, C_QC, C_GATE = 0, 1024, 1280, 1536, 1920, 2176, 2208, 3232
EPS = 1e-6
SBUF_BASE = 16512
SBUF_END = 229376


class Op:
    __slots__ = ("eng", "fn", "deps", "signal", "count", "grp", "gidx")


class Prog:
    ENGS = ("pe", "act", "dve", "pool", "sp")

    def __init__(self, nc):
        self.nc = nc
        self.ops = {e: [] for e in self.ENGS}
        self.last_w = {}
        self.readers = {}
        self.groups = {}

    def dma_group(self, name, n):
        self.groups[name] = dict(n=n, ops=[], sems=None)

    def _mk(self, eng, fn, grp):
        op = Op()
        op.eng = eng
        op.fn = fn
        op.signal = False
        op.count = None
        op.grp = grp
        op.gidx = None
        return op

    PSK = ("ps", "S", "O", "SUM", "po")

    def add(self, eng, fn, reads=(), writes=(), grp=None):
        pk = [k for k in reads if isinstance(k, tuple) and k[0] in self.PSK]
        if pk:
            reads = [k for k in reads if k not in pk]
            writes = list(writes) + pk
        op = self._mk(eng, fn, grp)
        deps = set()
        for k in reads:
            w = self.last_w.get(k)
            if w is not None:
                deps.add(w)
        for k in writes:
            w = self.last_w.get(k)
            if w is not None:
                deps.add(w)
            rd = self.readers.get(k)
            if rd:
                for r in rd.values():
                    if isinstance(r, list):
                        deps.update(r)
                    else:
                        deps.add(r)
        if grp is not None:
            g = self.groups[grp]
            i = len(g["ops"])
            op.gidx = i
            if i >= g["n"]:
                deps.add(g["ops"][i - g["n"]])
            g["ops"].append(op)
        if eng == "pe" and grp is None:
            deps = {d for d in deps if not (d.eng == "pe" and d.grp is None)}
        for d in deps:
            if d.grp is None:
                d.signal = True
        op.deps = deps
        for k in reads:
            rd = self.readers.setdefault(k, {})
            if grp is not None:
                rd.setdefault("dma", []).append(op)
            else:
                rd[eng] = op
        for k in writes:
            self.last_w[k] = op
            self.readers[k] = {}
        self.ops[eng].append(op)
        return op

    def barrier(self):
        lasts = []
        for e in self.ENGS:
            for op in reversed(self.ops[e]):
                if op.grp is None and op.fn is not None:
                    lasts.append(op)
                    break
        for g in self.groups.values():
            lasts.extend(g["ops"][-g["n"]:])
        for d in lasts:
            if d.grp is None:
                d.signal = True
        for e in self.ENGS:
            op = self._mk(e, None, None)
            op.deps = set(lasts)
            self.ops[e].append(op)
        self.last_w = {}
        self.readers = {}

    def wait_all(self, eng, ops):
        op = self._mk(eng, None, None)
        op.deps = set(ops)
        for d in ops:
            if d.grp is None:
                d.signal = True
        self.ops[eng].append(op)

    def emit(self):
        nc = self.nc
        for e in self.ENGS:
            c = 0
            for op in self.ops[e]:
                if op.grp is None and op.signal:
                    c += 1
                    op.count = c
        with contextlib.ExitStack() as st:
            esem = {e: st.enter_context(nc.semaphore("s_" + e)) for e in self.ENGS}
            for gname, g in self.groups.items():
                g["sems"] = [st.enter_context(nc.semaphore("g_%s_%d" % (gname, i))) for i in range(g["n"])]
            block = st.enter_context(nc.Block())
            prog = self

            def semval(d):
                if d.grp is not None:
                    g = prog.groups[d.grp]
                    return g["sems"][d.gidx % g["n"]], 16 * (d.gidx // g["n"] + 1)
                return esem[d.eng], d.count

            def run(ename, eng):
                known = {}
                init = getattr(prog, "init_" + ename, None)
                if init is not None:
                    init(eng)
                for op in prog.ops[ename]:
                    waits = {}
                    for d in op.deps:
                        s, v = semval(d)
                        key = id(s)
                        if key not in waits or waits[key][1] < v:
                            waits[key] = (s, v)
                    for key, (s, v) in waits.items():
                        if known.get(key, 0) >= v:
                            continue
                        eng.wait_ge(s, v)
                        known[key] = v
                    if op.fn is None:
                        continue
                    inst = op.fn(eng)
                    if op.grp is not None:
                        s, v = semval(op)
                        inst.then_inc(s, 16)
                    elif op.signal:
                        inst.then_inc(esem[ename], 1)

            @block.tensor
            def _(e):
                run("pe", e)

            @block.scalar
            def _(e):
                run("act", e)

            @block.vector
            def _(e):
                run("dve", e)

            @block.gpsimd
            def _(e):
                run("pool", e)

            @block.sync
            def _(e):
                run("sp", e)


class SB:
    def __init__(self, nc):
        self.nc = nc
        self.off = SBUF_BASE
        self.n = 0

    def alloc(self, shape, dt):
        esz = 4 if dt in (F32, I32) else 2
        sz = int(np.prod(shape[1:])) * esz
        sz = (sz + 63) // 64 * 64
        assert self.off + sz <= SBUF_END, ("SBUF overflow", self.off, sz)
        self.n += 1
        t = self.nc.alloc_sbuf_tensor_at("t%d" % self.n, list(shape), dt, offset=self.off)
        self.off += sz
        return t

    def mark(self):
        return self.off

    def reset(self, m):
        self.off = m


class _Stop(Exception):
    pass


def build(TP, depth, dbg=False, stop=None):
    nc = bass.Bass("TRN2", target_bir_lowering=False)
    NT = TP // 512
    NKT = TP // 128
    NKP = NKT // 2
    CB = max(1, (4 * TP // 64 + 127) // 128)
    CAP = CB * 128
    NSLOT = 64 * CAP
    L = depth

    def din(name, shape, dt=F32):
        return nc.dram_tensor(name, list(shape), dt, kind="ExternalInput").ap()

    def dscr(name, shape, dt):
        return nc.dram_tensor(name, list(shape), dt, kind=("ExternalOutput" if dbg else "Internal")).ap()

    x_in = din("x", [TP, D])
    mem_in = din("mem", [256, D])
    kbias_in = din("kbias", [128, NKP])
    tvalid_in = din("tvalid", [128, NKT])
    invbig_in = din("invbig", [128, NKT])
    eoff_in = din("eoff", [128, 64])
    cgT_in = din("cgT", [128, TP])
    sgT_in = din("sgT", [128, TP])
    cmT_in = din("cmT", [96, TP])
    smT_in = din("smT", [96, TP])
    identb_in = din("identb", [128, 128], BF16)
    identf_in = din("identf", [128, 128])
    onesb_in = din("onesb", [128, 128], BF16)
    onesf_in = din("onesf", [128, 128])
    ustr_in = din("ustr", [128, 128], BF16)
    rgT_in = din("rgT", [128, 128])
    rmT_in = din("rmT", [96, 96])
    rkT_in = din("rkT", [32, 32])
    w_in_d = din("w_in", [L, D, IN_W])
    w_qb_d = din("w_qb", [L, 384, 768])
    w_kvbk_d = din("w_kvbk", [L, 256, 512])
    w_kvbv_d = din("w_kvbv", [L, 256, 1024])
    memw_d = din("mem_w_kv", [L, D, 2048])
    w_out_d = din("w_out", [L, D, D])
    w_r_d = din("w_r", [L, D, 72])
    b_r_d = din("b_r", [L, 72])
    w_gu_d = din("w_gu", [L, 64, D, 512])
    w_dn_d = din("w_dn", [L, 64, 256, D])
    norm_mix_d = din("norm_mix", [L, D])
    norm_mem_d = din("norm_mem", [L, D])
    norm_ffn_d = din("norm_ffn", [L, D])
    gq_d = din("gqa_q_norm", [L, 128])
    gk_d = din("gqa_k_norm", [L, 128])
    gqa_d = din("mla_q_a_norm", [L, 384])
    gkva_d = din("mla_kv_a_norm", [L, 256])
    norm_final_d = din("norm_final", [D])
    y_out = nc.dram_tensor("y", [TP, D], F32, kind="ExternalOutput").ap()

    QgT = dscr("QgT", [8 * 128, TP], BF16)
    KgT = dscr("KgT", [2 * 128, TP], BF16)
    Vg = dscr("Vg", [2, 128, NKT, 128], BF16)
    QmT = dscr("QmT", [8 * 96, TP], BF16)
    KnT = dscr("KnT", [8 * 64, TP], BF16)
    KrT = dscr("KrT", [32, TP], BF16)
    Vm = dscr("Vm", [8, 128, NKT, 128], BF16)
    GT = dscr("GT", [2 * D, TP], BF16)
    GOT = dscr("GOT", [3 * D, TP], BF16)
    xmid = dscr("xmid", [TP, D], F32)
    xres = dscr("xres", [TP, D], F32)
    xs = dscr("xs", [NSLOT, D], BF16)
    ys = dscr("ys", [NSLOT, D], F32)

    P = Prog(nc)
    regs = {}

    def pool_init(eng):
        regs["bc"] = eng.alloc_register("bc")
        eng.reg_mov(regs["bc"], NSLOT - 1)

    P.init_pool = pool_init
    P.dma_group("ld", 8)
    P.dma_group("ld2", 4)
    P.dma_group("st", 8)
    P.dma_group("ind", 4)
    P.dma_group("stsp", 8)
    P.dma_group("lda", 6)
    P.dma_group("ldc", 6)
    sb = SB(nc)
    ps = nc.alloc_psum_tensor("ps", [128, 4096], F32)

    def chk(name):
        if stop == name:
            raise _Stop()

    def bank(i, n=1):
        return ps[:, i * 512:(i + n) * 512]

    cnt = {}

    def rot(name, n):
        c = cnt.get(name, 0)
        cnt[name] = c + 1
        return c % n

    def ld(out, in_, writes, reads=(), grp="ld", eng="sp", **kw):
        return P.add(eng, lambda e: e.dma_start(out=out, in_=in_, **kw), reads=reads, writes=writes, grp=grp)

    def st(out, in_, reads, writes=(), eng="pool", grp="st", **kw):
        return P.add(eng, lambda e: e.dma_start(out=out, in_=in_, **kw), reads=reads, writes=writes, grp=grp)

    def st_sp(out, in_, reads, writes=()):
        return st(out, in_, reads, writes, eng="sp", grp="stsp")

    def mm(out, lhsT, rhs, start, stop, reads, writes):
        return P.add("pe", lambda e: e.matmul(out, lhsT=lhsT, rhs=rhs, start=start, stop=stop), reads=reads, writes=writes)

    def tr(out, in_, ident, reads, writes):
        return P.add("pe", lambda e: e.transpose(out=out, in_=in_, identity=ident), reads=reads, writes=writes)

    def act(out, in_, func, reads, writes, **kw):
        return P.add("act", lambda e: e.activation(out=out, in_=in_, func=func, **kw), reads=reads, writes=writes)

    def tt(eng, out, in0, in1, op, reads, writes):
        return P.add(eng, lambda e: e.tensor_tensor(out=out, in0=in0, in1=in1, op=op), reads=reads, writes=writes)

    def ts(eng, out, in0, s1, s2, op0, op1, reads, writes):
        if op1 is None:
            return P.add(eng, lambda e: e.tensor_scalar(out=out, in0=in0, scalar1=s1, scalar2=None, op0=op0), reads=reads, writes=writes)
        return P.add(eng, lambda e: e.tensor_scalar(out=out, in0=in0, scalar1=s1, scalar2=s2, op0=op0, op1=op1), reads=reads, writes=writes)

    def stt(eng, out, in0, scalar, in1, op0, op1, reads, writes):
        return P.add(eng, lambda e: e.scalar_tensor_tensor(out=out, in0=in0, scalar=scalar, in1=in1, op0=op0, op1=op1), reads=reads, writes=writes)

    def cp(eng, out, in_, reads, writes):
        if eng == "act":
            return act(out, in_, AF.Copy, reads, writes)
        return P.add(eng, lambda e: e.tensor_copy(out=out, in_=in_), reads=reads, writes=writes)

    def recip(out, in_, reads, writes):
        return P.add("dve", lambda e: e.reciprocal(out=out, in_=in_), reads=reads, writes=writes)

    def rmax(out, in_, reads, writes):
        return P.add("dve", lambda e: e.reduce_max(out=out, in_=in_, axis=AX.X), reads=reads, writes=writes)

    def rsum(out, in_, reads, writes):
        return P.add("dve", lambda e: e.reduce_sum(out=out, in_=in_, axis=AX.X), reads=reads, writes=writes)

    identb = sb.alloc([128, 128], BF16)
    identf = sb.alloc([128, 128], F32)
    onesb = sb.alloc([128, 128], BF16)
    onesf = sb.alloc([128, 128], F32)
    ustr = sb.alloc([128, 128], BF16)
    rgT = sb.alloc([128, 128], F32)
    rmT = sb.alloc([128, 96], F32)
    rkT = sb.alloc([128, 32], F32)
    kbias = sb.alloc([128, NKP], F32)
    tvalid = sb.alloc([128, NKT], F32)
    invbig = sb.alloc([128, NKT], F32)
    eoff = sb.alloc([128, 64], F32)
    epst = sb.alloc([128, 1], F32)
    dest_i = sb.alloc([128, NKT, 2], I32)
    wts = sb.alloc([128, NKT, 2], F32)
    basec = sb.alloc([128, 64], F32)
    for t, src in ((identb, identb_in), (identf, identf_in), (onesb, onesb_in), (onesf, onesf_in), (ustr, ustr_in),
                   (rgT, rgT_in), (kbias, kbias_in), (tvalid, tvalid_in), (invbig, invbig_in), (eoff, eoff_in)):
        ld(t[:], src, ["const"])
    ld(rmT[0:96, :], rmT_in, ["const"])
    ld(rkT[0:32, :], rkT_in, ["const"])
    P.add("pool", lambda e: e.memset(epst[:], EPS), writes=["const"])

    pbase = sb.mark()
    chk("init")

    def load_cast(dst_fn, src_fn, ncols, stg, key, wkey):
        c0 = 0
        while c0 < ncols:
            w = min(2048, ncols - c0)
            ld(dst_fn(c0, w), src_fn(c0, w), [wkey], eng="pool", grp="ldc")
            c0 += w

    def rms_token_major(xt, xkey, hb, hbkey, msq, rstd, junk):
        act(junk[:], xt, AF.Square, [xkey], ["junk", "msq"], scale=1.0 / 32.0, accum_out=msq[:])
        act(rstd[:], msq[:], AF.Ln, ["msq", "const"], ["rstd"], bias=epst[:, 0:1], scale=1.0)
        act(rstd[:], rstd[:], AF.Exp, ["rstd"], ["rstd"], scale=-0.5)
        act(hb, xt, AF.Copy, [xkey, "rstd"], [hbkey], scale=rstd[:, 0:1])

    def phase_A(l, xsrc):
        sb.reset(pbase)
        g_mix = sb.alloc([128, 8], F32)
        g_mem = sb.alloc([128, 8], F32)
        g_q = sb.alloc([128, 1], F32)
        g_k = sb.alloc([128, 1], F32)
        g_qa = sb.alloc([128, 3], F32)
        g_kva = sb.alloc([128, 2], F32)
        ld(g_mix[:], norm_mix_d[l].rearrange("(c p) -> p c", p=128), ["gains"], allow_slow_non_contiguous=True)
        ld(g_mem[:], norm_mem_d[l].rearrange("(c p) -> p c", p=128), ["gains"], allow_slow_non_contiguous=True)
        ld(g_q[:], gq_d[l].rearrange("(c p) -> p c", p=128), ["gains"], allow_slow_non_contiguous=True)
        ld(g_k[:], gk_d[l].rearrange("(c p) -> p c", p=128), ["gains"], allow_slow_non_contiguous=True)
        ld(g_qa[:], gqa_d[l].rearrange("(c p) -> p c", p=128), ["gains"], allow_slow_non_contiguous=True)
        ld(g_kva[:], gkva_d[l].rearrange("(c p) -> p c", p=128), ["gains"], allow_slow_non_contiguous=True)
        mkT = sb.alloc([128, 8, 256], BF16)
        mv = sb.alloc([128, 2, 1024], BF16)
        w_qb_b = sb.alloc([128, 3, 768], BF16)
        w_kvbk_b = sb.alloc([128, 2, 512], BF16)
        w_kvbv_b = sb.alloc([128, 2, 1024], BF16)
        msq = sb.alloc([128, 1], F32)
        rstd = sb.alloc([128, 1], F32)
        junk = sb.alloc([128, 1024], BF16)
        psT = bank(0).bitcast(BF16)
        mA = sb.mark()
        stg = [sb.alloc([128, 2048], F32) for _ in range(2)]
        memw_b = sb.alloc([128, 8, 2048], BF16)
        memt = sb.alloc([128, 2, 1024], F32)
        membf = sb.alloc([128, 1024], BF16)
        memT = sb.alloc([128, 8, 256], BF16)
        mwv = memw_d[l].rearrange("(c p) n -> p c n", p=128)
        for c in range(8):
            load_cast(lambda c0, w, c=c: memw_b[:, c, c0:c0 + w], lambda c0, w, c=c: mwv[:, c, c0:c0 + w], 2048, stg, "stg", "memw")
        ld(memt[:], mem_in.rearrange("(kt p) d -> p kt d", p=128), ["memt"])
        for kt in range(2):
            rms_token_major(memt[:, kt, :], "memt", membf[:], "membf", msq, rstd, junk)
            for c in range(8):
                tr(psT[:, c * 128:(c + 1) * 128], membf[:, c * 128:(c + 1) * 128], identb[:], ["membf", "const"], [("ps", 0)])
            tt("dve", memT[:, :, kt * 128:(kt + 1) * 128], psT.rearrange("p (c n) -> p c n", c=8),
               g_mem[:].unsqueeze(2).to_broadcast([128, 8, 128]), ALU.mult, [("ps", 0), "gains"], ["memT"])
        for ch in range(8):
            b = 1 + rot("pj", 2)
            for kc in range(8):
                mm(bank(b)[:, 0:256], memw_b[:, kc, ch * 128:(ch + 1) * 128], memT[:, kc, :], kc == 0, kc == 7, ["memw", "memT"], [("ps", b)])
            cp("act", mkT[:, ch, :], bank(b)[:, 0:256], [("ps", b)], ["mkT"])
        for kt in range(2):
            for n in range(2):
                b = 1 + rot("pj", 2)
                for kc in range(8):
                    mm(bank(b), memT[:, kc, kt * 128:(kt + 1) * 128], memw_b[:, kc, 1024 + n * 512:1024 + (n + 1) * 512], kc == 0, kc == 7, ["memw", "memT"], [("ps", b)])
                cp("act", mv[:, kt, n * 512:(n + 1) * 512], bank(b), [("ps", b)], ["mv"])
        P.barrier()
        chk("A_mem")
        sb.reset(mA)
        w_in_b = sb.alloc([128, 8, IN_W], BF16)
        mW = sb.mark()
        stg = [sb.alloc([128, 2048], F32) for _ in range(2)]
        wv = w_in_d[l].rearrange("(c p) n -> p c n", p=128)
        for c in range(8):
            load_cast(lambda c0, w, c=c: w_in_b[:, c, c0:c0 + w], lambda c0, w, c=c: wv[:, c, c0:c0 + w], IN_W, stg, "stg", "w_in")
        v = w_qb_d[l].rearrange("(c p) n -> p c n", p=128)
        for c in range(3):
            load_cast(lambda c0, w, c=c: w_qb_b[:, c, c0:c0 + w], lambda c0, w, c=c: v[:, c, c0:c0 + w], 768, stg, "stg", "w_qb")
        v2 = w_kvbk_d[l].rearrange("(c p) n -> p c n", p=128)
        v3 = w_kvbv_d[l].rearrange("(c p) n -> p c n", p=128)
        for c in range(2):
            load_cast(lambda c0, w, c=c: w_kvbk_b[:, c, c0:c0 + w], lambda c0, w, c=c: v2[:, c, c0:c0 + w], 512, stg, "stg", "w_kvbk")
            load_cast(lambda c0, w, c=c: w_kvbv_b[:, c, c0:c0 + w], lambda c0, w, c=c: v3[:, c, c0:c0 + w], 1024, stg, "stg", "w_kvbv")
        P.barrier()
        chk("A_w")
        sb.reset(mW)
        xt = [sb.alloc([128, 1024], F32) for _ in range(2)]
        hb = [sb.alloc([128, 1024], BF16) for _ in range(2)]
        hT = sb.alloc([128, 8, 512], BF16)
        Cg = sb.alloc([128, 512], F32)
        Sg = sb.alloc([128, 512], F32)
        Cm = sb.alloc([128, 512], F32)
        Sm = sb.alloc([128, 512], F32)
        Ck = sb.alloc([128, 512], F32)
        Sk = sb.alloc([128, 512], F32)
        W = [dict(qsq=sb.alloc([128, 512], F32), rs=sb.alloc([128, 512], F32), qn=sb.alloc([128, 512], F32),
                  t1=sb.alloc([128, 512], F32), t2=sb.alloc([128, 512], F32)) for _ in range(2)]
        ob = [sb.alloc([128, 512], BF16) for _ in range(4)]
        qaf = sb.alloc([128, 3, 512], F32)
        qanT = sb.alloc([128, 3, 512], BF16)
        kvanT = sb.alloc([128, 2, 512], BF16)
        qcT = sb.alloc([128, 2, 512], BF16)
        gcT = sb.alloc([128, 2, 512], BF16)
        Pm = sb.alloc([128, 2, 512], BF16)
        rsm = sb.alloc([128, 512], F32)
        vst = [sb.alloc([128, 1024], BF16) for _ in range(2)]

        pend = []

        def defer(fn):
            if pend:
                pend.pop(0)()
            pend.append(fn)

        def flush():
            while pend:
                pend.pop(0)()

        def nob():
            i = rot("ob", 4)
            return ob[i], ("ob", i)

        def npj():
            b = 1 + rot("pj", 2)
            return bank(b), ("ps", b)

        def naux():
            b = 3 + rot("aux", 2)
            return bank(b), ("ps", b)

        def rope_out(src_f, srckey, np_, RT, Ctab, Stab, w, dst):
            aux, akey = bank(7), ("ps", 7)
            mm(aux[0:np_, :], RT, src_f, True, True, [srckey, "const"], [akey])
            tt("pool", w["t1"][0:np_, :], src_f, Ctab[0:np_, :], ALU.mult, [srckey, "tab"], [("t1", id(w))])
            tt("dve", w["t2"][0:np_, :], aux[0:np_, :], Stab[0:np_, :], ALU.mult, [akey, "tab"], [("t2", id(w))])
            o, okey = nob()
            tt("dve", o[0:np_, :], w["t1"][0:np_, :], w["t2"][0:np_, :], ALU.add, [("t1", id(w)), ("t2", id(w))], [okey])
            st_sp(dst, o[0:np_, :], [okey])

        for j in range(NT):
            t0 = j * 512
            tsl = slice(t0, t0 + 512)
            ld(Cg[:], cgT_in[:, tsl], ["tab"], grp="lda", eng="act")
            ld(Sg[:], sgT_in[:, tsl], ["tab"], grp="lda", eng="act")
            ld(Cm[0:96, :], cmT_in[:, tsl], ["tab"], grp="lda", eng="act")
            ld(Sm[0:96, :], smT_in[:, tsl], ["tab"], grp="lda", eng="act")
            ld(Ck[0:32, :], cmT_in[64:96, tsl], ["tab"], grp="lda", eng="act")
            ld(Sk[0:32, :], smT_in[64:96, tsl], ["tab"], grp="lda", eng="act")
            for s in range(4):
                i = rot("xt", 2)
                ld(xt[i][:], xsrc[t0 + s * 128:t0 + (s + 1) * 128, :], [("xt", i)], grp="lda", eng="act")
                rms_token_major(xt[i][:], ("xt", i), hb[i][:], ("hb", i), msq, rstd, junk)
                for c in range(8):
                    tr(psT[:, c * 128:(c + 1) * 128], hb[i][:, c * 128:(c + 1) * 128], identb[:], [("hb", i), "const"], [("ps", 0)])
                tt("dve", hT[:, :, s * 128:(s + 1) * 128], psT.rearrange("p (c n) -> p c n", c=8),
                   g_mix[:].unsqueeze(2).to_broadcast([128, 8, 128]), ALU.mult, [("ps", 0), "gains"], ["hT"])
            chk("A0")
            for hc in range(10):
                c0 = C_QG + hc * 128 if hc < 8 else C_KG + (hc - 8) * 128
                gain = g_q if hc < 8 else g_k
                dst = QgT[hc * 128:(hc + 1) * 128, tsl] if hc < 8 else KgT[(hc - 8) * 128:(hc - 7) * 128, tsl]
                w = W[rot("W", 2)]
                wk = id(w)
                pj, pkey = npj()
                for kc in range(8):
                    mm(pj, w_in_b[:, kc, c0:c0 + 128], hT[:, kc, :], kc == 0, kc == 7, ["w_in", "hT"], [pkey])
                act(w["qsq"][:], pj, AF.Square, [pkey], [("qsq", wk)])
                aux, akey = naux()
                mm(aux, onesf[:], w["qsq"][:], True, True, [("qsq", wk), "const"], [akey])

                def stage2(w=w, wk=wk, pj=pj, pkey=pkey, aux=aux, akey=akey, gain=gain, dst=dst):
                    act(w["rs"][:], aux, AF.Ln, [akey, "const"], [("rs", wk)], bias=epst[:, 0:1], scale=1.0 / 128.0)
                    act(w["rs"][:], w["rs"][:], AF.Exp, [("rs", wk)], [("rs", wk)], scale=-0.5)
                    stt("dve", w["qn"][:], pj, gain[:, 0:1], w["rs"][:], ALU.mult, ALU.mult, [pkey, ("rs", wk), "gains"], [("qn", wk)])
                    rope_out(w["qn"][:], ("qn", wk), 128, rgT[:], Cg, Sg, w, dst)

                defer(stage2)
            flush()
            chk("A1")
            for s in range(4):
                kt = j * 4 + s
                for kc in range(8):
                    mm(bank(7)[:, 0:256], hT[:, kc, s * 128:(s + 1) * 128], w_in_b[:, kc, C_VG:C_VG + 256], kc == 0, kc == 7, ["w_in", "hT"], [("ps", 7)])
                i = rot("vst", 2)
                cp("act", vst[i][:, 0:256], bank(7)[:, 0:256], [("ps", 7)], [("vst", i)])
                st_sp(Vg[:, :, kt, :].rearrange("g p d -> p g d"), vst[i][:, 0:256].rearrange("p (g d) -> p g d", g=2), [("vst", i)])
            chk("A2")
            for (cbase, nch, gain, dstT, dkey, dim) in ((C_QA, 3, g_qa, qanT, "qanT", 384.0), (C_KVA, 2, g_kva, kvanT, "kvanT", 256.0)):
                w = W[rot("W", 2)]
                wk = id(w)
                aux, akey = naux()
                for c in range(nch):
                    pj, pkey = npj()
                    for kc in range(8):
                        mm(pj, w_in_b[:, kc, cbase + c * 128:cbase + (c + 1) * 128], hT[:, kc, :], kc == 0, kc == 7, ["w_in", "hT"], [pkey])
                    cp("dve", qaf[:, c, :], pj, [pkey], [("qaf", c)])
                    sqn = ("qsq", "t1", "t2")[c]
                    act(w[sqn][:], pj, AF.Square, [pkey], [(sqn, wk)])
                for c in range(nch):
                    sqn = ("qsq", "t1", "t2")[c]
                    mm(aux, onesf[:], w[sqn][:], c == 0, c == nch - 1, [(sqn, wk), "const"], [akey])
                act(w["rs"][:], aux, AF.Ln, [akey, "const"], [("rs", wk)], bias=epst[:, 0:1], scale=1.0 / dim)
                act(w["rs"][:], w["rs"][:], AF.Exp, [("rs", wk)], [("rs", wk)], scale=-0.5)
                for c in range(nch):
                    stt("dve", dstT[:, c, :], qaf[:, c, :], gain[:, c:c + 1], w["rs"][:], ALU.mult, ALU.mult, [("qaf", c), ("rs", wk), "gains"], [dkey])
            chk("A4")
            w = W[rot("W", 2)]
            wk = id(w)
            pj, pkey = npj()
            for kc in range(8):
                mm(pj[0:32, :], w_in_b[:, kc, C_KR:C_KR + 32], hT[:, kc, :], kc == 0, kc == 7, ["w_in", "hT"], [pkey])
            cp("dve", w["qn"][0:32, :], pj[0:32, :], [pkey], [("qn", wk)])
            rope_out(w["qn"][0:32, :], ("qn", wk), 32, rkT[0:32, :], Ck, Sk, w, KrT[:, tsl])
            chk("A5")
            for h in range(8):
                w = W[rot("W", 2)]
                wk = id(w)
                pj, pkey = npj()
                for c in range(3):
                    mm(pj[0:96, :], w_qb_b[:, c, h * 96:(h + 1) * 96], qanT[:, c, :], c == 0, c == 2, ["w_qb", "qanT"], [pkey])
                cp("dve", w["qn"][0:96, :], pj[0:96, :], [pkey], [("qn", wk)])

                def stage2(w=w, wk=wk, h=h):
                    rope_out(w["qn"][0:96, :], ("qn", wk), 96, rmT[0:96, :], Cm, Sm, w, QmT[h * 96:(h + 1) * 96, tsl])

                defer(stage2)
            flush()
            chk("A6")
            for hp in range(4):
                pj, pkey = npj()
                for c in range(2):
                    mm(pj, w_kvbk_b[:, c, hp * 128:(hp + 1) * 128], kvanT[:, c, :], c == 0, c == 1, ["w_kvbk", "kvanT"], [pkey])
                o, okey = nob()
                cp("act", o[:], pj, [pkey], [okey])
                st_sp(KnT[hp * 128:(hp + 1) * 128, tsl], o[:], [okey])
            chk("A7")
            for s in range(4):
                kt = j * 4 + s
                i = rot("vst", 2)
                for n in range(2):
                    for c in range(2):
                        mm(bank(7), kvanT[:, c, s * 128:(s + 1) * 128], w_kvbv_b[:, c, n * 512:(n + 1) * 512], c == 0, c == 1, ["w_kvbv", "kvanT"], [("ps", 7)])
                    cp("act", vst[i][:, n * 512:(n + 1) * 512], bank(7), [("ps", 7)], [("vst", i)])
                st_sp(Vm[:, :, kt, :].rearrange("h p d -> p h d"), vst[i][:].rearrange("p (h d) -> p h d", h=8), [("vst", i)])
            chk("A8")
            for br in range(2):
                for c in range(8):
                    c0 = C_GATE + br * 1024 + c * 128
                    pj, pkey = npj()
                    for kc in range(8):
                        mm(pj, w_in_b[:, kc, c0:c0 + 128], hT[:, kc, :], kc == 0, kc == 7, ["w_in", "hT"], [pkey])
                    o, okey = nob()
                    act(o[:], pj, AF.Sigmoid, [pkey], [okey])
                    st_sp(GT[br * 1024 + c * 128:br * 1024 + (c + 1) * 128, tsl], o[:], [okey])
            chk("A9")
            for h in range(4):
                for dc in range(2):
                    c0 = C_QC + h * 256 + dc * 128
                    pj, pkey = npj()
                    for kc in range(8):
                        mm(pj, w_in_b[:, kc, c0:c0 + 128], hT[:, kc, :], kc == 0, kc == 7, ["w_in", "hT"], [pkey])
                    cp("dve", qcT[:, dc, :], pj, [pkey], [("qcT", dc)])
                for dc in range(2):
                    c0 = C_GATE + 2048 + h * 256 + dc * 128
                    pj, pkey = npj()
                    for kc in range(8):
                        mm(pj, w_in_b[:, kc, c0:c0 + 128], hT[:, kc, :], kc == 0, kc == 7, ["w_in", "hT"], [pkey])
                    act(gcT[:, dc, :], pj, AF.Sigmoid, [pkey], [("gcT", dc)])
                for kt in range(2):
                    for dc in range(2):
                        mm(bank(5 + kt), mkT[:, h * 2 + dc, kt * 128:(kt + 1) * 128], qcT[:, dc, :], dc == 0, dc == 1, ["mkT", ("qcT", dc)], [("ps", 5)])
                act(Pm[:], bank(5, 2).rearrange("p (k n) -> p k n", k=2), AF.Exp, [("ps", 5)], ["Pm"], scale=1.0 / 16.0)
                aux, akey = naux()
                for kt in range(2):
                    mm(aux, onesb[:], Pm[:, kt, :], kt == 0, kt == 1, ["Pm", "const"], [akey])
                recip(rsm[:], aux, [akey], ["rsm"])
                for dc in range(2):
                    pj, pkey = npj()
                    for kt in range(2):
                        mm(pj, mv[:, kt, h * 256 + dc * 128:h * 256 + (dc + 1) * 128], Pm[:, kt, :], kt == 0, kt == 1, ["mv", "Pm"], [pkey])
                    w = W[rot("W", 2)]
                    wk = id(w)
                    tt("dve", w["t2"][:], pj, rsm[:], ALU.mult, [pkey, "rsm"], [("t2", wk)])
                    o, okey = nob()
                    tt("pool", o[:], w["t2"][:], gcT[:, dc, :], ALU.mult, [("t2", wk), ("gcT", dc)], [okey])
                    r0 = 2 * 1024 + (h * 2 + dc) * 128
                    st_sp(GOT[r0:r0 + 128, tsl], o[:], [okey])
        P.barrier()

    def phase_B(l):
        sb.reset(pbase)
        Kt = [sb.alloc([128, TP], BF16) for _ in range(2)]
        Vt = [sb.alloc([128, NKT, 128], BF16) for _ in range(2)]
        Qt = [sb.alloc([128, 512], BF16) for _ in range(2)]
        Gt = [sb.alloc([128, 512], BF16) for _ in range(2)]
        Pb = [sb.alloc([128, 2, 512], BF16) for _ in range(3)]
        rs = [sb.alloc([128, 512], F32) for _ in range(2)]
        of = [sb.alloc([128, 512], F32) for _ in range(2)]
        ob = [sb.alloc([128, 512], BF16) for _ in range(2)]
        if l == 0:
            zt = sb.alloc([128, 4096], BF16)
            P.add("pool", lambda e: e.memset(zt[:], 0.0), writes=["zt"])
            xs_v = xs.rearrange("(a p r) d -> a p (r d)", p=128, r=4)
            for a in range(NSLOT // 512):
                st(xs_v[a], zt[:], ["zt"])
        s1 = [sb.alloc([128, 512], BF16) for _ in range(2)]
        s2 = [sb.alloc([128, 512], BF16) for _ in range(2)]
        assert NKP % 2 == 0
        heads = []
        for g in range(2):
            for hh in range(4):
                h = g * 4 + hh
                heads.append(dict(kind="g", kv=g, kd=128, scale=128.0 ** -0.5, q=QgT[h * 128:(h + 1) * 128, :],
                                  gt=GT[h * 128:(h + 1) * 128, :], dst=GOT[h * 128:(h + 1) * 128, :], first=(hh == 0)))
        for h in range(8):
            heads.append(dict(kind="m", kv=h, kd=96, scale=96.0 ** -0.5, q=QmT[h * 96:(h + 1) * 96, :],
                              gt=GT[1024 + h * 128:1024 + (h + 1) * 128, :], dst=GOT[1024 + h * 128:1024 + (h + 1) * 128, :], first=True))
        steps = []
        for hi, hd in enumerate(heads):
            for j in range(NT):
                for kp in range(NKP):
                    steps.append((hi, j, kp))
        state = {"kv": None}

        def load_kv(hd):
            i = rot("kv", 2)
            if hd["kind"] == "g":
                g = hd["kv"]
                nsp = max(1, TP // 2048)
                for a in range(nsp):
                    sl = slice(a * (TP // nsp), (a + 1) * (TP // nsp))
                    ld(Kt[i][:, sl], KgT[g * 128:(g + 1) * 128, sl], [("K", i)])
                for a in range(nsp):
                    sl = slice(a * (NKT // nsp), (a + 1) * (NKT // nsp))
                    ld(Vt[i][:, sl, :], Vg[g, :, sl, :], [("V", i)])
            else:
                h = hd["kv"]
                nsp = max(1, TP // 2048)
                for a in range(nsp):
                    sl = slice(a * (TP // nsp), (a + 1) * (TP // nsp))
                    ld(Kt[i][0:64, sl], KnT[h * 64:(h + 1) * 64, sl], [("K", i)])
                    ld(Kt[i][64:96, sl], KrT[:, sl], [("K", i)])
                for a in range(nsp):
                    sl = slice(a * (NKT // nsp), (a + 1) * (NKT // nsp))
                    ld(Vt[i][:, sl, :], Vm[h, :, sl, :], [("V", i)])
            return i

        cur = {}

        def emit_qk(step):
            hi, j, kp = step
            hd = heads[hi]
            if j == 0 and kp == 0 and hd["first"]:
                if state.get("pre") is not None:
                    state["kv"] = state["pre"]
                    state["pre"] = None
                else:
                    state["kv"] = load_kv(hd)
            if j == NT - 1 and kp == 0 and hi + 1 < len(heads) and heads[hi + 1]["first"] and NT > 1:
                state["pre"] = load_kv(heads[hi + 1])
            kvi = state["kv"]
            kd = hd["kd"]
            if kp == 0:
                qi = rot("Q", 2)
                ld(Qt[qi][0:kd, :], hd["q"][:, j * 512:(j + 1) * 512], [("Q", qi)])
                ld(Gt[qi][:], hd["gt"][:, j * 512:(j + 1) * 512], [("G", qi)])
                cur[(hi, j)] = dict(qi=qi, kvi=kvi, ob=rot("O", 2))
            c = cur[(hi, j)]
            sbuf = rot("S", 3)
            c[("s", kp)] = sbuf
            for t in range(2):
                kt = kp * 2 + t
                mm(bank(sbuf * 2 + t), Kt[c["kvi"]][0:kd, kt * 128:(kt + 1) * 128], Qt[c["qi"]][0:kd, :], True, True,
                   [("K", c["kvi"]), ("Q", c["qi"])], [("S", sbuf)])

        def emit_pv(step):
            hi, j, kp = step
            hd = heads[hi]
            c = cur[(hi, j)]
            sbuf = c[("s", kp)]
            pi = rot("P", 3)
            act(Pb[pi][:], bank(sbuf * 2, 2).rearrange("p (k n) -> p k n", k=2), AF.Exp, [("S", sbuf), "const"], [("P", pi)],
                bias=kbias[:, kp:kp + 1], scale=hd["scale"])
            o = c["ob"]
            for t in range(2):
                kt = kp * 2 + t
                first = (kp == 0 and t == 0)
                last = (kp == NKP - 1 and t == 1)
                mm(bank(6), Vt[c["kvi"]][:, kt, :], Pb[pi][:, t, :], first, last, [("V", c["kvi"]), ("P", pi)], [("O", 0)])
            a = kp % 2

            def issue_sum(pend):
                b2_, first_, last_ = pend
                mm(bank(7), onesb[:], s2[b2_][:], first_, last_, [("s2", b2_), "const"], [("SUM", 0)])

            if a == 0 and c.get("pend") is not None:
                issue_sum(c["pend"])
                c["pend"] = None
            tt("dve", s1[a][:], Pb[pi][:, 0, :], Pb[pi][:, 1, :], ALU.add, [("P", pi)], [("s1", a)])
            if a == 1:
                b2 = rot("s2", 2)
                tt("dve", s2[b2][:], s1[0][:], s1[1][:], ALU.add, [("s1", 0), ("s1", 1)], [("s2", b2)])
                pend = (b2, kp == 1, kp == NKP - 1)
                if kp == NKP - 1:
                    issue_sum(pend)
                else:
                    c["pend"] = pend
            if kp == NKP - 1:
                recip(rs[o][:], bank(7), [("SUM", 0)], [("rs", o)])
                tt("dve", of[o][:], bank(6), rs[o][:], ALU.mult, [("O", 0), ("rs", o)], [("of", o)])
                tt("pool", ob[o][:], of[o][:], Gt[c["qi"]][:], ALU.mult, [("of", o), ("G", c["qi"])], [("ob", o)])
                st(hd["dst"][:, j * 512:(j + 1) * 512], ob[o][:], [("ob", o)])
                del cur[(hi, j)]

        emit_qk(steps[0])
        emit_qk(steps[1])
        for si in range(len(steps)):
            if si + 2 < len(steps):
                emit_qk(steps[si + 2])
            emit_pv(steps[si])
        P.barrier()

    def phase_C(l, xsrc, last):
        sb.reset(pbase)
        w_out_b = sb.alloc([128, 8, D], BF16)
        g_ffn = sb.alloc([128, D], F32)
        w_r = sb.alloc([128, 8, 72], F32)
        b_r = sb.alloc([128, 72], F32)
        mC = sb.mark()
        stg = [sb.alloc([128, 2048], F32) for _ in range(2)]
        wv = w_out_d[l].rearrange("(c p) n -> p c n", p=128)
        for c in range(8):
            load_cast(lambda c0, w, c=c: w_out_b[:, c, c0:c0 + w], lambda c0, w, c=c: wv[:, c, c0:c0 + w], D, stg, "stg", "w_out")
        ld(g_ffn[:], norm_ffn_d[l].partition_broadcast(128), ["g_ffn"])
        ld(w_r[:], w_r_d[l].rearrange("(c p) n -> p c n", p=128), ["w_r"])
        ld(b_r[:], b_r_d[l].partition_broadcast(128), ["b_r"])
        P.add("pool", lambda e: e.memset(basec[:], 0.0), writes=["basec"])
        P.barrier()
        sb.reset(mC)
        got = [sb.alloc([128, 24, 512], BF16) for _ in range(2)]
        xt = [sb.alloc([128, D], F32) for _ in range(2)]
        xn = [sb.alloc([128, D], F32) for _ in range(2)]
        h2s = [sb.alloc([128, D], F32) for _ in range(2)]
        h2b = [sb.alloc([128, D], BF16) for _ in range(2)]
        pendc = []

        def deferc(fn):
            if pendc:
                pendc.pop(0)()
            pendc.append(fn)
        h2T = sb.alloc([128, 8, 128], F32)
        junk = sb.alloc([128, D], BF16)
        msq = sb.alloc([128, 1], F32)
        rstd = sb.alloc([128, 1], F32)
        lg = sb.alloc([128, 72], F32)
        sm = {k: sb.alloc([128, 1], F32) for k in ("gmax", "ngmax", "gsum", "m1", "m2", "d21", "e21", "den", "d1", "d2")}
        ge = sb.alloc([128, 8], F32)
        ohg = sb.alloc([128, 8], F32)
        pen = sb.alloc([128, 8], F32)
        elm = sb.alloc([128, 64], F32)
        elm2 = sb.alloc([128, 64], F32)
        oh1 = sb.alloc([128, 64], F32)
        oh2 = sb.alloc([128, 64], F32)
        Asum = sb.alloc([128, 64], F32)
        Ab = sb.alloc([128, 64], BF16)
        rank = sb.alloc([128, 64], F32)
        slot = sb.alloc([128, 64], F32)
        ovf = sb.alloc([128, 64], F32)
        tmp = sb.alloc([128, 64], F32)
        BIG = 1.0e4
        BIGIDX = 1.0e6
        GOTv = GOT.rearrange("(bc p) t -> p bc t", p=128)
        for j in range(NT):
            gi = rot("got", 2)
            for b3 in range(3):
                ld(got[gi][:, b3 * 8:(b3 + 1) * 8, :], GOTv[:, b3 * 8:(b3 + 1) * 8, j * 512:(j + 1) * 512], [("got", gi)])
            for s in range(4):
                tile = j * 4 + s
                r0 = tile * 128
                xi = rot("xt", 2)
                ld(xt[xi][:], xsrc[r0:r0 + 128, :], [("xt", xi)])
                pb = rot("po", 2) * 2
                for n in range(2):
                    for bc in range(24):
                        mm(bank(pb + n), got[gi][:, bc, s * 128:(s + 1) * 128], w_out_b[:, bc % 8, n * 512:(n + 1) * 512], bc == 0, bc == 23,
                           [("got", gi), "w_out"], [("po", pb)])
                tt("dve", xn[xi][:], bank(pb, 2), xt[xi][:], ALU.add, [("po", pb), ("xt", xi)], [("xn", xi)])
                st(xmid[r0:r0 + 128, :], xn[xi][:], [("xn", xi)])
                act(junk[:], xn[xi][:], AF.Square, [("xn", xi)], ["junk", "msq"], scale=1.0 / 32.0, accum_out=msq[:])
                act(rstd[:], msq[:], AF.Sqrt, ["msq", "const"], ["rstd"], bias=epst[:, 0:1], scale=1.0)
                recip(rstd[:], rstd[:], ["rstd"], ["rstd"])
                hq = tile % 2
                h2 = h2s[hq]
                stt("dve", h2[:], xn[xi][:], rstd[:, 0:1], g_ffn[:], ALU.mult, ALU.mult, [("xn", xi), "rstd", "g_ffn"], [("h2", hq)])
                hi_ = rot("h2b", 2)
                cp("act", h2b[hi_][:], h2[:], [("h2", hq)], [("h2b", hi_)])

                def stage2(tile=tile, hi_=hi_, hq=hq, h2=h2):
                    for c in range(8):
                        tr(bank(4, 2)[:, c * 128:(c + 1) * 128], h2[:, c * 128:(c + 1) * 128], identf[:], [("h2", hq), "const"], [("ps", 4)])
                    cp("dve", h2T[:], bank(4, 2).rearrange("p (c n) -> p c n", c=8), [("ps", 4)], ["h2T"])
                    for c in range(8):
                        mm(bank(6)[:, 0:72], h2T[:, c, :], w_r[:, c, :], c == 0, c == 7, ["h2T", "w_r"], [("ps", 6)])
                    tt("dve", lg[:], bank(6)[:, 0:72], b_r[:], ALU.add, [("ps", 6), "b_r"], ["lg"])
                    rmax(sm["gmax"][:], lg[:, 0:8], ["lg"], ["gmax"])
                    ts("dve", sm["ngmax"][:], sm["gmax"][:], -1.0, None, ALU.mult, None, ["gmax"], ["ngmax"])
                    act(ge[:], lg[:, 0:8], AF.Exp, ["lg", "ngmax"], ["ge", "gsum"], bias=sm["ngmax"][:, 0:1], scale=1.0, accum_out=sm["gsum"][:])
                    ts("dve", ohg[:], lg[:, 0:8], sm["gmax"][:, 0:1], None, ALU.is_ge, None, ["lg", "gmax"], ["ohg"])
                    ts("dve", pen[:], ohg[:], -1.0, BIG, ALU.add, ALU.mult, ["ohg"], ["pen"])
                    tt("dve", elm[:].rearrange("p (g e) -> p g e", g=8), lg[:, 8:72].rearrange("p (g e) -> p g e", g=8),
                       pen[:].unsqueeze(2).to_broadcast([128, 8, 8]), ALU.add, ["lg", "pen"], ["elm"])
                    rmax(sm["m1"][:], elm[:], ["elm"], ["m1"])
                    ts("dve", oh1[:], elm[:], sm["m1"][:, 0:1], None, ALU.is_ge, None, ["elm", "m1"], ["oh1"])
                    stt("dve", elm2[:], oh1[:], -BIG, elm[:], ALU.mult, ALU.add, ["oh1", "elm"], ["elm2"])
                    rmax(sm["m2"][:], elm2[:], ["elm2"], ["m2"])
                    ts("dve", oh2[:], elm2[:], sm["m2"][:, 0:1], None, ALU.is_ge, None, ["elm2", "m2"], ["oh2"])
                    tt("dve", sm["d21"][:], sm["m2"][:], sm["m1"][:], ALU.subtract, ["m1", "m2"], ["d21"])
                    act(sm["e21"][:], sm["d21"][:], AF.Exp, ["d21"], ["e21"])
                    ts("dve", sm["den"][:], sm["e21"][:], 1.0, sm["gsum"][:, 0:1], ALU.add, ALU.mult, ["e21", "gsum"], ["den"])
                    recip(wts[:, tile, 0:1], sm["den"][:], ["den"], [("wts", tile)])
                    tt("dve", wts[:, tile, 1:2], wts[:, tile, 0:1], sm["e21"][:], ALU.mult, [("wts", tile), "e21"], [("wts", tile)])
                    tt("dve", Asum[:], oh1[:], oh2[:], ALU.add, ["oh1", "oh2"], ["Asum"])
                    ts("dve", Ab[:], Asum[:], tvalid[:, tile:tile + 1], None, ALU.mult, None, ["Asum", "const"], ["Ab"])
                    mm(bank(7)[:, 0:64], ustr[:], Ab[:], True, True, ["Ab", "const"], [("ps", 7)])
                    mm(bank(7)[:, 64:128], onesb[:], Ab[:], True, True, ["Ab", "const"], [("ps", 7)])
                    tt("dve", rank[:], bank(7)[:, 0:64], basec[:], ALU.add, [("ps", 7), "basec"], ["rank"])
                    tt("dve", basec[:], bank(7)[:, 64:128], basec[:], ALU.add, [("ps", 7), "basec"], ["basec"])
                    ts("dve", ovf[:], rank[:], float(CAP), BIGIDX, ALU.is_ge, ALU.mult, ["rank"], ["ovf"])
                    tt("dve", slot[:], rank[:], eoff[:], ALU.add, ["rank", "const"], ["slot"])
                    tt("dve", slot[:], slot[:], ovf[:], ALU.add, ["slot", "ovf"], ["slot"])
                    for k, oh, dk in ((0, oh1, "d1"), (1, oh2, "d2")):
                        tt("dve", tmp[:], oh[:], slot[:], ALU.mult, ["oh1", "oh2", "slot"], ["tmp"])
                        rsum(sm[dk][:], tmp[:], ["tmp"], [dk])
                        ts("dve", sm[dk][:], sm[dk][:], invbig[:, tile:tile + 1], None, ALU.add, None, [dk, "const"], [dk])
                        cp("dve", dest_i[:, tile, k:k + 1], sm[dk][:], [dk], [("dest", tile)])
                        P.add("pool", lambda e, k=k, tile=tile, hi_=hi_: e.indirect_dma_start(
                            out=xs, out_offset=bass.IndirectOffsetOnAxis(ap=dest_i[:, tile, k:k + 1], axis=0),
                            in_=h2b[hi_][:], in_offset=None, bounds_check=regs["bc"], oob_is_err=False),
                            reads=[("h2b", hi_), ("dest", tile)], writes=[], grp="ind")

                deferc(stage2)
        while pendc:
            pendc.pop(0)()
        P.barrier()
        chk("C1")
        sb.reset(pbase)
        CR = CAP
        wgb = [sb.alloc([128, 8, 512], BF16) for _ in range(2)]
        wdb = [sb.alloc([128, 2, D], BF16) for _ in range(2)]
        xsb = [sb.alloc([128, CB, D], BF16) for _ in range(2)]
        xbT = sb.alloc([128, 8, CR], BF16)
        sil = sb.alloc([128, 2, CR], F32)
        actT = sb.alloc([128, 2, CR], BF16)
        yo = [sb.alloc([128, D], F32) for _ in range(2)]
        def c2_loads(e_):
            wi = e_ % 2
            gv = w_gu_d[l, e_].rearrange("(c p) n -> p c n", p=128)
            for hlf in range(2):
                ld(wgb[wi][:, hlf * 4:(hlf + 1) * 4, :], gv[:, hlf * 4:(hlf + 1) * 4, :], [("wgb", wi)], eng="pool", grp="ldc")
            ld(wdb[wi][:], w_dn_d[l, e_].rearrange("(c p) n -> p c n", p=128), [("wdb", wi)], eng="pool", grp="ldc")
            ld(xsb[wi][:], xs[e_ * CAP:(e_ + 1) * CAP, :].rearrange("(b p) d -> p b d", p=128), [("xsb", wi)])

        c2_loads(0)
        for e_ in range(64):
            wi = e_ % 2
            if e_ + 1 < 64:
                c2_loads(e_ + 1)
            for b in range(CB):
                tb = rot("tb", 2)
                pT = bank(tb).bitcast(BF16)
                for c in range(8):
                    tr(pT[:, c * 128:(c + 1) * 128], xsb[wi][:, b, c * 128:(c + 1) * 128], identb[:], [("xsb", wi), "const"], [("ps", tb)])
                cp("dve", xbT[:, :, b * 128:(b + 1) * 128], pT.rearrange("p (c n) -> p c n", c=8), [("ps", tb)], ["xbT"])
            for ch in range(4):
                for c in range(8):
                    mm(bank(2 + ch)[:, 0:CR], wgb[wi][:, c, ch * 128:(ch + 1) * 128], xbT[:, c, :], c == 0, c == 7, [("wgb", wi), "xbT"], [("ps", 2 + ch)])
            for fc in range(2):
                act(sil[:, fc, :], bank(2 + fc)[:, 0:CR], AF.Silu, [("ps", 2 + fc)], [("sil", fc)])
                tt("dve", actT[:, fc, :], sil[:, fc, :], bank(4 + fc)[:, 0:CR], ALU.mult, [("sil", fc), ("ps", 4 + fc)], ["actT"])
            for b in range(CB):
                db = 6 if b % 2 == 0 else 0
                dkeys = [("ps", db), ("ps", db + 1)]
                for n in range(2):
                    for fc in range(2):
                        mm(bank(db + n), actT[:, fc, b * 128:(b + 1) * 128], wdb[wi][:, fc, n * 512:(n + 1) * 512], fc == 0, fc == 1, ["actT", ("wdb", wi)], dkeys)
                yi = rot("yo", 2)
                cp("act" if b % 2 == 0 else "dve", yo[yi][:], bank(db, 2), dkeys, [("yo", yi)])
                r0 = e_ * CAP + b * 128
                st_sp(ys[r0:r0 + 128, :], yo[yi][:], [("yo", yi)])
        P.barrier()
        chk("C2")
        sb.reset(pbase)
        y0 = [sb.alloc([128, D], F32) for _ in range(2)]
        y1 = [sb.alloc([128, D], F32) for _ in range(2)]
        xn = [sb.alloc([128, D], F32) for _ in range(2)]
        acc = [sb.alloc([128, D], F32) for _ in range(2)]
        outt = [sb.alloc([128, D], F32) for _ in range(2)]
        g_fin = sb.alloc([128, D], F32)
        junk = sb.alloc([128, D], BF16)
        msq = sb.alloc([128, 1], F32)
        rstd = sb.alloc([128, 1], F32)
        ld(g_fin[:], norm_final_d.partition_broadcast(128), ["g_fin"])
        for i in range(2):
            P.add("pool", lambda e, i=i: e.memset(y0[i][:], 0.0), writes=[("y0", i)])
            P.add("pool", lambda e, i=i: e.memset(y1[i][:], 0.0), writes=[("y1", i)])
        for tile in range(NKT):
            r0 = tile * 128
            i = rot("c3", 2)
            ld(xn[i][:], xmid[r0:r0 + 128, :], [("xn", i)])
            for k, yy, yk in ((0, y0, "y0"), (1, y1, "y1")):
                P.add("pool", lambda e, k=k, yy=yy, i=i, tile=tile: e.indirect_dma_start(
                    out=yy[i][:], out_offset=None, in_=ys,
                    in_offset=bass.IndirectOffsetOnAxis(ap=dest_i[:, tile, k:k + 1], axis=0),
                    bounds_check=regs["bc"], oob_is_err=False), reads=[], writes=[(yk, i)], grp="ind")
            stt("dve", acc[i][:], y0[i][:], wts[:, tile, 0:1], xn[i][:], ALU.mult, ALU.add, [("y0", i), ("xn", i)], [("acc", i)])
            if not last:
                stt("dve", outt[i][:], y1[i][:], wts[:, tile, 1:2], acc[i][:], ALU.mult, ALU.add, [("y1", i), ("acc", i)], [("outt", i)])
                st(xres[r0:r0 + 128, :], outt[i][:], [("outt", i)])
            else:
                stt("dve", acc[i][:], y1[i][:], wts[:, tile, 1:2], acc[i][:], ALU.mult, ALU.add, [("y1", i), ("acc", i)], [("acc", i)])
                act(junk[:], acc[i][:], AF.Square, [("acc", i)], ["junk", "msq"], scale=1.0 / 32.0, accum_out=msq[:])
                act(rstd[:], msq[:], AF.Sqrt, ["msq", "const"], ["rstd"], bias=epst[:, 0:1], scale=1.0)
                recip(rstd[:], rstd[:], ["rstd"], ["rstd"])
                stt("dve", outt[i][:], acc[i][:], rstd[:, 0:1], g_fin[:], ALU.mult, ALU.mult, [("acc", i), "rstd", "g_fin"], [("outt", i)])
                st(y_out[r0:r0 + 128, :], outt[i][:], [("outt", i)])
        P.barrier()

    xsrc = x_in
    try:
        for l in range(L):
            phase_A(l, xsrc)
            chk("A")
            phase_B(l)
            chk("B")
            phase_C(l, xsrc, last=(l == L - 1))
            xsrc = xres
    except _Stop:
        P.barrier()
    P.emit()
    return nc


def _rope_tables(TP, rot_dim):
    rows = TP // 64
    row_idx = np.repeat(np.arange(rows, dtype=np.float32), 64)
    col_idx = np.tile(np.arange(64, dtype=np.float32), rows)
    n_freq = rot_dim // 4
    inv_freq = (np.float32(10000.0) ** (-np.arange(n_freq, dtype=np.float32) / np.float32(n_freq))).astype(np.float32)
    ang = np.concatenate([row_idx[:, None] * inv_freq, col_idx[:, None] * inv_freq], axis=-1).astype(np.float32)
    return np.cos(ang).astype(np.float32), np.sin(ang).astype(np.float32)


def _rot_T(n, base, half):
    R = np.zeros((n, n), np.float32)
    for i in range(half):
        R[base + i, base + i + half] = -1.0
        R[base + i + half, base + i] = 1.0
    return np.ascontiguousarray(R.T)


def make_consts(TP, CAP):
    bf = ml_dtypes.bfloat16
    cg, sg = _rope_tables(TP, 128)
    cm, sm = _rope_tables(TP, 32)
    c = {}
    c["cgT"] = np.ascontiguousarray(np.concatenate([cg, cg], axis=1).T)
    c["sgT"] = np.ascontiguousarray(np.concatenate([sg, sg], axis=1).T)
    cmT = np.ones((96, TP), np.float32)
    smT = np.zeros((96, TP), np.float32)
    cmT[64:96] = np.concatenate([cm, cm], axis=1).T
    smT[64:96] = np.concatenate([sm, sm], axis=1).T
    c["cmT"] = cmT
    c["smT"] = smT
    c["identb"] = np.eye(128, dtype=np.float32).astype(bf)
    c["identf"] = np.eye(128, dtype=np.float32)
    c["onesb"] = np.ones((128, 128), np.float32).astype(bf)
    c["onesf"] = np.ones((128, 128), np.float32)
    c["ustr"] = np.triu(np.ones((128, 128), np.float32), 1).astype(bf)
    c["rgT"] = _rot_T(128, 0, 64)
    c["rmT"] = _rot_T(96, 64, 16)
    c["rkT"] = _rot_T(32, 0, 16)
    c["eoff"] = np.ascontiguousarray(np.broadcast_to((np.arange(64, dtype=np.float32) * CAP)[None, :], (128, 64)))
    return c


def core_inputs(x, mem, S, TP, shared):
    NKT = TP // 128
    NKP = NKT // 2
    xp = np.zeros((TP, D), np.float32)
    xp[:S] = x
    d = dict(shared)
    d["x"] = xp
    d["mem"] = np.ascontiguousarray(mem, dtype=np.float32)
    kb = np.where(np.arange(NKP) * 256 < S, 0.0, -30000.0).astype(np.float32)
    d["kbias"] = np.ascontiguousarray(np.broadcast_to(kb[None, :], (128, NKP)))
    tok = np.arange(NKT)[None, :] * 128 + np.arange(128)[:, None]
    valid = (tok < S).astype(np.float32)
    d["tvalid"] = valid
    d["invbig"] = ((1.0 - valid) * 1.0e6).astype(np.float32)
    return d


def shared_inputs(TP, CAP, w):
    s = make_consts(TP, CAP)
    f = lambda a: np.ascontiguousarray(np.asarray(a, dtype=np.float32))
    s["w_in"] = f(w["w_in"])
    s["w_qb"] = f(w["mla_w_qb"])
    kvb = f(w["mla_w_kvb"]).reshape(-1, 256, 8, 192)
    s["w_kvbk"] = np.ascontiguousarray(kvb[:, :, :, :64].reshape(-1, 256, 512))
    s["w_kvbv"] = np.ascontiguousarray(kvb[:, :, :, 64:].reshape(-1, 256, 1024))
    s["mem_w_kv"] = f(w["mem_w_kv"])
    s["w_out"] = f(w["w_out"])
    s["w_r"] = np.ascontiguousarray(np.concatenate([f(w["w_group"]), f(w["w_expert"])], axis=-1))
    s["b_r"] = np.ascontiguousarray(np.concatenate([f(w["b_group"]), f(w["b_expert"])], axis=-1))
    s["w_gu"] = f(w["w_gate_up"])
    s["w_dn"] = f(w["w_down"])
    for k in ("norm_mix", "norm_mem", "norm_ffn", "gqa_q_norm", "gqa_k_norm", "mla_q_a_norm", "mla_kv_a_norm", "norm_final"):
        s[k] = f(w[k])
    return s


def kernel(x_prompt, x_sample, mem_prompt, mem_sample, **w):
    TP = 8192
    depth = 2
    CAP = max(1, (4 * TP // 64 + 127) // 128) * 128
    x_prompt = np.asarray(x_prompt)
    x_sample = np.asarray(x_sample)
    mem_prompt = np.asarray(mem_prompt)
    mem_sample = np.asarray(mem_sample)
    shared = shared_inputs(TP, CAP, w)
    in_maps = []
    for b in range(4):
        in_maps.append(core_inputs(x_prompt[b], mem_prompt[b], 4096, TP, shared))
    for b in range(4):
        in_maps.append(core_inputs(x_sample[b], mem_sample[b], 8192, TP, shared))
    nc = build(TP, depth)
    res = run_bass_kernel_spmd(nc, in_maps, core_ids=list(range(8)))
    yp = np.stack([np.asarray(res.results[b]["y"])[:4096] for b in range(4)]).astype(np.float32)
    ysm = np.stack([np.asarray(res.results[4 + b]["y"]) for b in range(4)]).astype(np.float32)
    return (yp, ysm)
```

```python
import contextlib
import numpy as np
import ml_dtypes
import concourse.bass as bass
import concourse.mybir as mybir
from concourse.bass_utils import run_bass_kernel_spmd

F32 = mybir.dt.float32
BF16 = mybir.dt.bfloat16
I32 = mybir.dt.int32
AF = mybir.ActivationFunctionType
ALU = mybir.AluOpType
AX = mybir.AxisListType

D = 1024
IN_W = 6304
C_QG, C_KG, C_VG, C_QA, C_KVA, C_KR, C_QC, C_GATE = 0, 1024, 1280, 1536, 1920, 2176, 2208, 3232
EPS = 1e-6
SBUF_BASE = 16512
SBUF_END = 229376


class Op:
    __slots__ = ("eng", "fn", "deps", "signal", "count", "grp", "gidx")


class Prog:
    ENGS = ("pe", "act", "dve", "pool", "sp")

    def __init__(self, nc):
        self.nc = nc
        self.ops = {e: [] for e in self.ENGS}
        self.last_w = {}
        self.readers = {}
        self.groups = {}

    def dma_group(self, name, n):
        self.groups[name] = dict(n=n, ops=[], sems=None)

    def _mk(self, eng, fn, grp):
        op = Op()
        op.eng = eng
        op.fn = fn
        op.signal = False
        op.count = None
        op.grp = grp
        op.gidx = None
        return op

    PSK = ("ps", "S", "O", "SUM", "po")

    def add(self, eng, fn, reads=(), writes=(), grp=None):
        pk = [k for k in reads if isinstance(k, tuple) and k[0] in self.PSK]
        if pk:
            reads = [k for k in reads if k not in pk]
            writes = list(writes) + pk
        op = self._mk(eng, fn, grp)
        deps = set()
        for k in reads:
            w = self.last_w.get(k)
            if w is not None:
                deps.add(w)
        for k in writes:
            w = self.last_w.get(k)
            if w is not None:
                deps.add(w)
            rd = self.readers.get(k)
            if rd:
                for r in rd.values():
                    if isinstance(r, list):
                        deps.update(r)
                    else:
                        deps.add(r)
        if grp is not None:
            g = self.groups[grp]
            i = len(g["ops"])
            op.gidx = i
            if i >= g["n"]:
                deps.add(g["ops"][i - g["n"]])
            g["ops"].append(op)
        if eng == "pe" and grp is None:
            deps = {d for d in deps if not (d.eng == "pe" and d.grp is None)}
        for d in deps:
            if d.grp is None:
                d.signal = True
        op.deps = deps
        for k in reads:
            rd = self.readers.setdefault(k, {})
            if grp is not None:
                rd.setdefault("dma", []).append(op)
            else:
                rd[eng] = op
        for k in writes:
            self.last_w[k] = op
            self.readers[k] = {}
        self.ops[eng].append(op)
        return op

    def barrier(self):
        lasts = []
        for e in self.ENGS:
            for op in reversed(self.ops[e]):
                if op.grp is None and op.fn is not None:
                    lasts.append(op)
                    break
        for g in self.groups.values():
            lasts.extend(g["ops"][-g["n"]:])
        for d in lasts:
            if d.grp is None:
                d.signal = True
        for e in self.ENGS:
            op = self._mk(e, None, None)
            op.deps = set(lasts)
            self.ops[e].append(op)
        self.last_w = {}
        self.readers = {}

    def wait_all(self, eng, ops):
        op = self._mk(eng, None, None)
        op.deps = set(ops)
        for d in ops:
            if d.grp is None:
                d.signal = True
        self.ops[eng].append(op)

    def emit(self):
        nc = self.nc
        for e in self.ENGS:
            c = 0
            for op in self.ops[e]:
                if op.grp is None and op.signal:
                    c += 1
                    op.count = c
        with contextlib.ExitStack() as st:
            esem = {e: st.enter_context(nc.semaphore("s_" + e)) for e in self.ENGS}
            for gname, g in self.groups.items():
                g["sems"] = [st.enter_context(nc.semaphore("g_%s_%d" % (gname, i))) for i in range(g["n"])]
            block = st.enter_context(nc.Block())
            prog = self

            def semval(d):
                if d.grp is not None:
                    g = prog.groups[d.grp]
                    return g["sems"][d.gidx % g["n"]], 16 * (d.gidx // g["n"] + 1)
                return esem[d.eng], d.count

            def run(ename, eng):
                known = {}
                init = getattr(prog, "init_" + ename, None)
                if init is not None:
                    init(eng)
                for op in prog.ops[ename]:
                    waits = {}
                    for d in op.deps:
                        s, v = semval(d)
                        key = id(s)
                        if key not in waits or waits[key][1] < v:
                            waits[key] = (s, v)
                    for key, (s, v) in waits.items():
                        if known.get(key, 0) >= v:
                            continue
                        eng.wait_ge(s, v)
                        known[key] = v
                    if op.fn is None:
                        continue
                    inst = op.fn(eng)
                    if op.grp is not None:
                        s, v = semval(op)
                        inst.then_inc(s, 16)
                    elif op.signal:
                        inst.then_inc(esem[ename], 1)

            @block.tensor
            def _(e):
                run("pe", e)

            @block.scalar
            def _(e):
                run("act", e)

            @block.vector
            def _(e):
                run("dve", e)

            @block.gpsimd
            def _(e):
                run("pool", e)

            @block.sync
            def _(e):
                run("sp", e)


class SB:
    def __init__(self, nc):
        self.nc = nc
        self.off = SBUF_BASE
        self.n = 0

    def alloc(self, shape, dt):
        esz = 4 if dt in (F32, I32) else 2
        sz = int(np.prod(shape[1:])) * esz
        sz = (sz + 63) // 64 * 64
        assert self.off + sz <= SBUF_END, ("SBUF overflow", self.off, sz)
        self.n += 1
        t = self.nc.alloc_sbuf_tensor_at("t%d" % self.n, list(shape), dt, offset=self.off)
        self.off += sz
        return t

    def mark(self):
        return self.off

    def reset(self, m):
        self.off = m


class _Stop(Exception):
    pass


def build(TP, depth, dbg=False, stop=None):
    nc = bass.Bass("TRN2", target_bir_lowering=False)
    NT = TP // 512
    NKT = TP // 128
    NKP = NKT // 2
    CB = max(1, (4 * TP // 64 + 127) // 128)
    CAP = CB * 128
    NSLOT = 64 * CAP
    L = depth

    def din(name, shape, dt=F32):
        return nc.dram_tensor(name, list(shape), dt, kind="ExternalInput").ap()

    def dscr(name, shape, dt):
        return nc.dram_tensor(name, list(shape), dt, kind=("ExternalOutput" if dbg else "Internal")).ap()

    x_in = din("x", [TP, D])
    mem_in = din("mem", [256, D])
    kbias_in = din("kbias", [128, NKP])
    tvalid_in = din("tvalid", [128, NKT])
    invbig_in = din("invbig", [128, NKT])
    eoff_in = din("eoff", [128, 64])
    cgT_in = din("cgT", [128, TP])
    sgT_in = din("sgT", [128, TP])
    cmT_in = din("cmT", [96, TP])
    smT_in = din("smT", [96, TP])
    identb_in = din("identb", [128, 128], BF16)
    identf_in = din("identf", [128, 128])
    onesb_in = din("onesb", [128, 128], BF16)
    onesf_in = din("onesf", [128, 128])
    ustr_in = din("ustr", [128, 128], BF16)
    rgT_in = din("rgT", [128, 128])
    rmT_in = din("rmT", [96, 96])
    rkT_in = din("rkT", [32, 32])
    w_in_d = din("w_in", [L, D, IN_W])
    w_qb_d = din("w_qb", [L, 384, 768])
    w_kvbk_d = din("w_kvbk", [L, 256, 512])
    w_kvbv_d = din("w_kvbv", [L, 256, 1024])
    memw_d = din("mem_w_kv", [L, D, 2048])
    w_out_d = din("w_out", [L, D, D])
    w_r_d = din("w_r", [L, D, 72])
    b_r_d = din("b_r", [L, 72])
    w_gu_d = din("w_gu", [L, 64, D, 512])
    w_dn_d = din("w_dn", [L, 64, 256, D])
    norm_mix_d = din("norm_mix", [L, D])
    norm_mem_d = din("norm_mem", [L, D])
    norm_ffn_d = din("norm_ffn", [L, D])
    gq_d = din("gqa_q_norm", [L, 128])
    gk_d = din("gqa_k_norm", [L, 128])
    gqa_d = din("mla_q_a_norm", [L, 384])
    gkva_d = din("mla_kv_a_norm", [L, 256])
    norm_final_d = din("norm_final", [D])
    y_out = nc.dram_tensor("y", [TP, D], F32, kind="ExternalOutput").ap()

    QgT = dscr("QgT", [8 * 128, TP], BF16)
    KgT = dscr("KgT", [2 * 128, TP], BF16)
    Vg = dscr("Vg", [2, 128, NKT, 128], BF16)
    QmT = dscr("QmT", [8 * 96, TP], BF16)
    KnT = dscr("KnT", [8 * 64, TP], BF16)
    KrT = dscr("KrT", [32, TP], BF16)
    Vm = dscr("Vm", [8, 128, NKT, 128], BF16)
    GT = dscr("GT", [2 * D, TP], BF16)
    GOT = dscr("GOT", [3 * D, TP], BF16)
    xmid = dscr("xmid", [TP, D], F32)
    xres = dscr("xres", [TP, D], F32)
    xs = dscr("xs", [NSLOT, D], BF16)
    ys = dscr("ys", [NSLOT, D], F32)

    P = Prog(nc)
    regs = {}

    def pool_init(eng):
        regs["bc"] = eng.alloc_register("bc")
        eng.reg_mov(regs["bc"], NSLOT - 1)

    P.init_pool = pool_init
    P.dma_group("ld", 8)
    P.dma_group("ld2", 4)
    P.dma_group("st", 8)
    P.dma_group("ind", 4)
    P.dma_group("stsp", 8)
    P.dma_group("lda", 6)
    P.dma_group("ldc", 6)
    sb = SB(nc)
    ps = nc.alloc_psum_tensor("ps", [128, 4096], F32)

    def chk(name):
        if stop == name:
            raise _Stop()

    def bank(i, n=1):
        return ps[:, i * 512:(i + n) * 512]

    cnt = {}

    def rot(name, n):
        c = cnt.get(name, 0)
        cnt[name] = c + 1
        return c % n

    def ld(out, in_, writes, reads=(), grp="ld", eng="sp", **kw):
        return P.add(eng, lambda e: e.dma_start(out=out, in_=in_, **kw), reads=reads, writes=writes, grp=grp)

    def st(out, in_, reads, writes=(), eng="pool", grp="st", **kw):
        return P.add(eng, lambda e: e.dma_start(out=out, in_=in_, **kw), reads=reads, writes=writes, grp=grp)

    def st_sp(out, in_, reads, writes=()):
        return st(out, in_, reads, writes, eng="sp", grp="stsp")

    def mm(out, lhsT, rhs, start, stop, reads, writes):
        return P.add("pe", lambda e: e.matmul(out, lhsT=lhsT, rhs=rhs, start=start, stop=stop), reads=reads, writes=writes)

    def tr(out, in_, ident, reads, writes):
        return P.add("pe", lambda e: e.transpose(out=out, in_=in_, identity=ident), reads=reads, writes=writes)

    def act(out, in_, func, reads, writes, **kw):
        return P.add("act", lambda e: e.activation(out=out, in_=in_, func=func, **kw), reads=reads, writes=writes)

    def tt(eng, out, in0, in1, op, reads, writes):
        return P.add(eng, lambda e: e.tensor_tensor(out=out, in0=in0, in1=in1, op=op), reads=reads, writes=writes)

    def ts(eng, out, in0, s1, s2, op0, op1, reads, writes):
        if op1 is None:
            return P.add(eng, lambda e: e.tensor_scalar(out=out, in0=in0, scalar1=s1, scalar2=None, op0=op0), reads=reads, writes=writes)
        return P.add(eng, lambda e: e.tensor_scalar(out=out, in0=in0, scalar1=s1, scalar2=s2, op0=op0, op1=op1), reads=reads, writes=writes)

    def stt(eng, out, in0, scalar, in1, op0, op1, reads, writes):
        return P.add(eng, lambda e: e.scalar_tensor_tensor(out=out, in0=in0, scalar=scalar, in1=in1, op0=op0, op1=op1), reads=reads, writes=writes)

    def cp(eng, out, in_, reads, writes):
        if eng == "act":
            return act(out, in_, AF.Copy, reads, writes)
        return P.add(eng, lambda e: e.tensor_copy(out=out, in_=in_), reads=reads, writes=writes)

    def recip(out, in_, reads, writes):
        return P.add("dve", lambda e: e.reciprocal(out=out, in_=in_), reads=reads, writes=writes)

    def rmax(out, in_, reads, writes):
        return P.add("dve", lambda e: e.reduce_max(out=out, in_=in_, axis=AX.X), reads=reads, writes=writes)

    def rsum(out, in_, reads, writes):
        return P.add("dve", lambda e: e.reduce_sum(out=out, in_=in_, axis=AX.X), reads=reads, writes=writes)

    identb = sb.alloc([128, 128], BF16)
    identf = sb.alloc([128, 128], F32)
    onesb = sb.alloc([128, 128], BF16)
    onesf = sb.alloc([128, 128], F32)
    ustr = sb.alloc([128, 128], BF16)
    rgT = sb.alloc([128, 128], F32)
    rmT = sb.alloc([128, 96], F32)
    rkT = sb.alloc([128, 32], F32)
    kbias = sb.alloc([128, NKP], F32)
    tvalid = sb.alloc([128, NKT], F32)
    invbig = sb.alloc([128, NKT], F32)
    eoff = sb.alloc([128, 64], F32)
    epst = sb.alloc([128, 1], F32)
    dest_i = sb.alloc([128, NKT, 2], I32)
    wts = sb.alloc([128, NKT, 2], F32)
    basec = sb.alloc([128, 64], F32)
    for t, src in ((identb, identb_in), (identf, identf_in), (onesb, onesb_in), (onesf, onesf_in), (ustr, ustr_in),
                   (rgT, rgT_in), (kbias, kbias_in), (tvalid, tvalid_in), (invbig, invbig_in), (eoff, eoff_in)):
        ld(t[:], src, ["const"])
    ld(rmT[0:96, :], rmT_in, ["const"])
    ld(rkT[0:32, :], rkT_in, ["const"])
    P.add("pool", lambda e: e.memset(epst[:], EPS), writes=["const"])

    pbase = sb.mark()
    chk("init")

    def load_cast(dst_fn, src_fn, ncols, stg, key, wkey):
        c0 = 0
        while c0 < ncols:
            w = min(2048, ncols - c0)
            ld(dst_fn(c0, w), src_fn(c0, w), [wkey], eng="pool", grp="ldc")
            c0 += w

    def rms_token_major(xt, xkey, hb, hbkey, msq, rstd, junk):
        act(junk[:], xt, AF.Square, [xkey], ["junk", "msq"], scale=1.0 / 32.0, accum_out=msq[:])
        act(rstd[:], msq[:], AF.Ln, ["msq", "const"], ["rstd"], bias=epst[:, 0:1], scale=1.0)
        act(rstd[:], rstd[:], AF.Exp, ["rstd"], ["rstd"], scale=-0.5)
        act(hb, xt, AF.Copy, [xkey, "rstd"], [hbkey], scale=rstd[:, 0:1])

    def phase_A(l, xsrc):
        sb.reset(pbase)
        g_mix = sb.alloc([128, 8], F32)
        g_mem = sb.alloc([128, 8], F32)
        g_q = sb.alloc([128, 1], F32)
        g_k = sb.alloc([128, 1], F32)
        g_qa = sb.alloc([128, 3], F32)
        g_kva = sb.alloc([128, 2], F32)
        ld(g_mix[:], norm_mix_d[l].rearrange("(c p) -> p c", p=128), ["gains"], allow_slow_non_contiguous=True)
        ld(g_mem[:], norm_mem_d[l].rearrange("(c p) -> p c", p=128), ["gains"], allow_slow_non_contiguous=True)
        ld(g_q[:], gq_d[l].rearrange("(c p) -> p c", p=128), ["gains"], allow_slow_non_contiguous=True)
        ld(g_k[:], gk_d[l].rearrange("(c p) -> p c", p=128), ["gains"], allow_slow_non_contiguous=True)
        ld(g_qa[:], gqa_d[l].rearrange("(c p) -> p c", p=128), ["gains"], allow_slow_non_contiguous=True)
        ld(g_kva[:], gkva_d[l].rearrange("(c p) -> p c", p=128), ["gains"], allow_slow_non_contiguous=True)
        mkT = sb.alloc([128, 8, 256], BF16)
        mv = sb.alloc([128, 2, 1024], BF16)
        w_qb_b = sb.alloc([128, 3, 768], BF16)
        w_kvbk_b = sb.alloc([128, 2, 512], BF16)
        w_kvbv_b = sb.alloc([128, 2, 1024], BF16)
        msq = sb.alloc([128, 1], F32)
        rstd = sb.alloc([128, 1], F32)
        junk = sb.alloc([128, 1024], BF16)
        psT = bank(0).bitcast(BF16)
        mA = sb.mark()
        stg = [sb.alloc([128, 2048], F32) for _ in range(2)]
        memw_b = sb.alloc([128, 8, 2048], BF16)
        memt = sb.alloc([128, 2, 1024], F32)
        membf = sb.alloc([128, 1024], BF16)
        memT = sb.alloc([128, 8, 256], BF16)
        mwv = memw_d[l].rearrange("(c p) n -> p c n", p=128)
        for c in range(8):
            load_cast(lambda c0, w, c=c: memw_b[:, c, c0:c0 + w], lambda c0, w, c=c: mwv[:, c, c0:c0 + w], 2048, stg, "stg", "memw")
        ld(memt[:], mem_in.rearrange("(kt p) d -> p kt d", p=128), ["memt"])
        for kt in range(2):
            rms_token_major(memt[:, kt, :], "memt", membf[:], "membf", msq, rstd, junk)
            for c in range(8):
                tr(psT[:, c * 128:(c + 1) * 128], membf[:, c * 128:(c + 1) * 128], identb[:], ["membf", "const"], [("ps", 0)])
            tt("dve", memT[:, :, kt * 128:(kt + 1) * 128], psT.rearrange("p (c n) -> p c n", c=8),
               g_mem[:].unsqueeze(2).to_broadcast([128, 8, 128]), ALU.mult, [("ps", 0), "gains"], ["memT"])
        for ch in range(8):
            b = 1 + rot("pj", 2)
            for kc in range(8):
                mm(bank(b)[:, 0:256], memw_b[:, kc, ch * 128:(ch + 1) * 128], memT[:, kc, :], kc == 0, kc == 7, ["memw", "memT"], [("ps", b)])
            cp("act", mkT[:, ch, :], bank(b)[:, 0:256], [("ps", b)], ["mkT"])
        for kt in range(2):
            for n in range(2):
                b = 1 + rot("pj", 2)
                for kc in range(8):
                    mm(bank(b), memT[:, kc, kt * 128:(kt + 1) * 128], memw_b[:, kc, 1024 + n * 512:1024 + (n + 1) * 512], kc == 0, kc == 7, ["memw", "memT"], [("ps", b)])
                cp("act", mv[:, kt, n * 512:(n + 1) * 512], bank(b), [("ps", b)], ["mv"])
        P.barrier()
        chk("A_mem")
        sb.reset(mA)
        w_in_b = sb.alloc([128, 8, IN_W], BF16)
        mW = sb.mark()
        stg = [sb.alloc([128, 2048], F32) for _ in range(2)]
        wv = w_in_d[l].rearrange("(c p) n -> p c n", p=128)
        for c in range(8):
            load_cast(lambda c0, w, c=c: w_in_b[:, c, c0:c0 + w], lambda c0, w, c=c: wv[:, c, c0:c0 + w], IN_W, stg, "stg", "w_in")
        v = w_qb_d[l].rearrange("(c p) n -> p c n", p=128)
        for c in range(3):
            load_cast(lambda c0, w, c=c: w_qb_b[:, c, c0:c0 + w], lambda c0, w, c=c: v[:, c, c0:c0 + w], 768, stg, "stg", "w_qb")
        v2 = w_kvbk_d[l].rearrange("(c p) n -> p c n", p=128)
        v3 = w_kvbv_d[l].rearrange("(c p) n -> p c n", p=128)
        for c in range(2):
            load_cast(lambda c0, w, c=c: w_kvbk_b[:, c, c0:c0 + w], lambda c0, w, c=c: v2[:, c, c0:c0 + w], 512, stg, "stg", "w_kvbk")
            load_cast(lambda c0, w, c=c: w_kvbv_b[:, c, c0:c0 + w], lambda c0, w, c=c: v3[:, c, c0:c0 + w], 1024, stg, "stg", "w_kvbv")
        P.barrier()
        chk("A_w")
        sb.reset(mW)
        xt = [sb.alloc([128, 1024], F32) for _ in range(2)]
        hb = [sb.alloc([128, 1024], BF16) for _ in range(2)]
        hT = sb.alloc([128, 8, 512], BF16)
        Cg = sb.alloc([128, 512], F32)
        Sg = sb.alloc([128, 512], F32)
        Cm = sb.alloc([128, 512], F32)
        Sm = sb.alloc([128, 512], F32)
        Ck = sb.alloc([128, 512], F32)
        Sk = sb.alloc([128, 512], F32)
        W = [dict(qsq=sb.alloc([128, 512], F32), rs=sb.alloc([128, 512], F32), qn=sb.alloc([128, 512], F32),
                  t1=sb.alloc([128, 512], F32), t2=sb.alloc([128, 512], F32)) for _ in range(2)]
        ob = [sb.alloc([128, 512], BF16) for _ in range(4)]
        qaf = sb.alloc([128, 3, 512], F32)
        qanT = sb.alloc([128, 3, 512], BF16)
        kvanT = sb.alloc([128, 2, 512], BF16)
        qcT = sb.alloc([128, 2, 512], BF16)
        gcT = sb.alloc([128, 2, 512], BF16)
        Pm = sb.alloc([128, 2, 512], BF16)
        rsm = sb.alloc([128, 512], F32)
        vst = [sb.alloc([128, 1024], BF16) for _ in range(2)]

        pend = []

        def defer(fn):
            if pend:
                pend.pop(0)()
            pend.append(fn)

        def flush():
            while pend:
                pend.pop(0)()

        def nob():
            i = rot("ob", 4)
            return ob[i], ("ob", i)

        def npj():
            b = 1 + rot("pj", 2)
            return bank(b), ("ps", b)

        def naux():
            b = 3 + rot("aux", 2)
            return bank(b), ("ps", b)

        def rope_out(src_f, srckey, np_, RT, Ctab, Stab, w, dst):
            aux, akey = bank(7), ("ps", 7)
            mm(aux[0:np_, :], RT, src_f, True, True, [srckey, "const"], [akey])
            tt("pool", w["t1"][0:np_, :], src_f, Ctab[0:np_, :], ALU.mult, [srckey, "tab"], [("t1", id(w))])
            tt("dve", w["t2"][0:np_, :], aux[0:np_, :], Stab[0:np_, :], ALU.mult, [akey, "tab"], [("t2", id(w))])
            o, okey = nob()
            tt("dve", o[0:np_, :], w["t1"][0:np_, :], w["t2"][0:np_, :], ALU.add, [("t1", id(w)), ("t2", id(w))], [okey])
            st_sp(dst, o[0:np_, :], [okey])

        for j in range(NT):
            t0 = j * 512
            tsl = slice(t0, t0 + 512)
            ld(Cg[:], cgT_in[:, tsl], ["tab"], grp="lda", eng="act")
            ld(Sg[:], sgT_in[:, tsl], ["tab"], grp="lda", eng="act")
            ld(Cm[0:96, :], cmT_in[:, tsl], ["tab"], grp="lda", eng="act")
            ld(Sm[0:96, :], smT_in[:, tsl], ["tab"], grp="lda", eng="act")
            ld(Ck[0:32, :], cmT_in[64:96, tsl], ["tab"], grp="lda", eng="act")
            ld(Sk[0:32, :], smT_in[64:96, tsl], ["tab"], grp="lda", eng="act")
            for s in range(4):
                i = rot("xt", 2)
                ld(xt[i][:], xsrc[t0 + s * 128:t0 + (s + 1) * 128, :], [("xt", i)], grp="lda", eng="act")
                rms_token_major(xt[i][:], ("xt", i), hb[i][:], ("hb", i), msq, rstd, junk)
                for c in range(8):
                    tr(psT[:, c * 128:(c + 1) * 128], hb[i][:, c * 128:(c + 1) * 128], identb[:], [("hb", i), "const"], [("ps", 0)])
                tt("dve", hT[:, :, s * 128:(s + 1) * 128], psT.rearrange("p (c n) -> p c n", c=8),
                   g_mix[:].unsqueeze(2).to_broadcast([128, 8, 128]), ALU.mult, [("ps", 0), "gains"], ["hT"])
            chk("A0")
            for hc in range(10):
                c0 = C_QG + hc * 128 if hc < 8 else C_KG + (hc - 8) * 128
                gain = g_q if hc < 8 else g_k
                dst = QgT[hc * 128:(hc + 1) * 128, tsl] if hc < 8 else KgT[(hc - 8) * 128:(hc - 7) * 128, tsl]
                w = W[rot("W", 2)]
                wk = id(w)
                pj, pkey = npj()
                for kc in range(8):
                    mm(pj, w_in_b[:, kc, c0:c0 + 128], hT[:, kc, :], kc == 0, kc == 7, ["w_in", "hT"], [pkey])
                act(w["qsq"][:], pj, AF.Square, [pkey], [("qsq", wk)])
                aux, akey = naux()
                mm(aux, onesf[:], w["qsq"][:], True, True, [("qsq", wk), "const"], [akey])

                def stage2(w=w, wk=wk, pj=pj, pkey=pkey, aux=aux, akey=akey, gain=gain, dst=dst):
                    act(w["rs"][:], aux, AF.Ln, [akey, "const"], [("rs", wk)], bias=epst[:, 0:1], scale=1.0 / 128.0)
                    act(w["rs"][:], w["rs"][:], AF.Exp, [("rs", wk)], [("rs", wk)], scale=-0.5)
                    stt("dve", w["qn"][:], pj, gain[:, 0:1], w["rs"][:], ALU.mult, ALU.mult, [pkey, ("rs", wk), "gains"], [("qn", wk)])
                    rope_out(w["qn"][:], ("qn", wk), 128, rgT[:], Cg, Sg, w, dst)

                defer(stage2)
            flush()
            chk("A1")
            for s in range(4):
                kt = j * 4 + s
                for kc in range(8):
                    mm(bank(7)[:, 0:256], hT[:, kc, s * 128:(s + 1) * 128], w_in_b[:, kc, C_VG:C_VG + 256], kc == 0, kc == 7, ["w_in", "hT"], [("ps", 7)])
                i = rot("vst", 2)
                cp("act", vst[i][:, 0:256], bank(7)[:, 0:256], [("ps", 7)], [("vst", i)])
                st_sp(Vg[:, :, kt, :].rearrange("g p d -> p g d"), vst[i][:, 0:256].rearrange("p (g d) -> p g d", g=2), [("vst", i)])
            chk("A2")
            for (cbase, nch, gain, dstT, dkey, dim) in ((C_QA, 3, g_qa, qanT, "qanT", 384.0), (C_KVA, 2, g_kva, kvanT, "kvanT", 256.0)):
                w = W[rot("W", 2)]
                wk = id(w)
                aux, akey = naux()
                for c in range(nch):
                    pj, pkey = npj()
                    for kc in range(8):
                        mm(pj, w_in_b[:, kc, cbase + c * 128:cbase + (c + 1) * 128], hT[:, kc, :], kc == 0, kc == 7, ["w_in", "hT"], [pkey])
                    cp("dve", qaf[:, c, :], pj, [pkey], [("qaf", c)])
                    sqn = ("qsq", "t1", "t2")[c]
                    act(w[sqn][:], pj, AF.Square, [pkey], [(sqn, wk)])
                for c in range(nch):
                    sqn = ("qsq", "t1", "t2")[c]
                    mm(aux, onesf[:], w[sqn][:], c == 0, c == nch - 1, [(sqn, wk), "const"], [akey])
                act(w["rs"][:], aux, AF.Ln, [akey, "const"], [("rs", wk)], bias=epst[:, 0:1], scale=1.0 / dim)
                act(w["rs"][:], w["rs"][:], AF.Exp, [("rs", wk)], [("rs", wk)], scale=-0.5)
                for c in range(nch):
                    stt("dve", dstT[:, c, :], qaf[:, c, :], gain[:, c:c + 1], w["rs"][:], ALU.mult, ALU.mult, [("qaf", c), ("rs", wk), "gains"], [dkey])
            chk("A4")
            w = W[rot("W", 2)]
            wk = id(w)
            pj, pkey = npj()
            for kc in range(8):
                mm(pj[0:32, :], w_in_b[:, kc, C_KR:C_KR + 32], hT[:, kc, :], kc == 0, kc == 7, ["w_in", "hT"], [pkey])
            cp("dve", w["qn"][0:32, :], pj[0:32, :], [pkey], [("qn", wk)])
            rope_out(w["qn"][0:32, :], ("qn", wk), 32, rkT[0:32, :], Ck, Sk, w, KrT[:, tsl])
            chk("A5")
            for h in range(8):
                w = W[rot("W", 2)]
                wk = id(w)
                pj, pkey = npj()
                for c in range(3):
                    mm(pj[0:96, :], w_qb_b[:, c, h * 96:(h + 1) * 96], qanT[:, c, :], c == 0, c == 2, ["w_qb", "qanT"], [pkey])
                cp("dve", w["qn"][0:96, :], pj[0:96, :], [pkey], [("qn", wk)])

                def stage2(w=w, wk=wk, h=h):
                    rope_out(w["qn"][0:96, :], ("qn", wk), 96, rmT[0:96, :], Cm, Sm, w, QmT[h * 96:(h + 1) * 96, tsl])

                defer(stage2)
            flush()
            chk("A6")
            for hp in range(4):
                pj, pkey = npj()
                for c in range(2):
                    mm(pj, w_kvbk_b[:, c, hp * 128:(hp + 1) * 128], kvanT[:, c, :], c == 0, c == 1, ["w_kvbk", "kvanT"], [pkey])
                o, okey = nob()
                cp("act", o[:], pj, [pkey], [okey])
                st_sp(KnT[hp * 128:(hp + 1) * 128, tsl], o[:], [okey])
            chk("A7")
            for s in range(4):
                kt = j * 4 + s
                i = rot("vst", 2)
                for n in range(2):
                    for c in range(2):
                        mm(bank(7), kvanT[:, c, s * 128:(s + 1) * 128], w_kvbv_b[:, c, n * 512:(n + 1) * 512], c == 0, c == 1, ["w_kvbv", "kvanT"], [("ps", 7)])
                    cp("act", vst[i][:, n * 512:(n + 1) * 512], bank(7), [("ps", 7)], [("vst", i)])
                st_sp(Vm[:, :, kt, :].rearrange("h p d -> p h d"), vst[i][:].rearrange("p (h d) -> p h d", h=8), [("vst", i)])
            chk("A8")
            for br in range(2):
                for c in range(8):
                    c0 = C_GATE + br * 1024 + c * 128
                    pj, pkey = npj()
                    for kc in range(8):
                        mm(pj, w_in_b[:, kc, c0:c0 + 128], hT[:, kc, :], kc == 0, kc == 7, ["w_in", "hT"], [pkey])
                    o, okey = nob()
                    act(o[:], pj, AF.Sigmoid, [pkey], [okey])
                    st_sp(GT[br * 1024 + c * 128:br * 1024 + (c + 1) * 128, tsl], o[:], [okey])
            chk("A9")
            for h in range(4):
                for dc in range(2):
                    c0 = C_QC + h * 256 + dc * 128
                    pj, pkey = npj()
                    for kc in range(8):
                        mm(pj, w_in_b[:, kc, c0:c0 + 128], hT[:, kc, :], kc == 0, kc == 7, ["w_in", "hT"], [pkey])
                    cp("dve", qcT[:, dc, :], pj, [pkey], [("qcT", dc)])
                for dc in range(2):
                    c0 = C_GATE + 2048 + h * 256 + dc * 128
                    pj, pkey = npj()
                    for kc in range(8):
                        mm(pj, w_in_b[:, kc, c0:c0 + 128], hT[:, kc, :], kc == 0, kc == 7, ["w_in", "hT"], [pkey])
                    act(gcT[:, dc, :], pj, AF.Sigmoid, [pkey], [("gcT", dc)])
                for kt in range(2):
                    for dc in range(2):
                        mm(bank(5 + kt), mkT[:, h * 2 + dc, kt * 128:(kt + 1) * 128], qcT[:, dc, :], dc == 0, dc == 1, ["mkT", ("qcT", dc)], [("ps", 5)])
                act(Pm[:], bank(5, 2).rearrange("p (k n) -> p k n", k=2), AF.Exp, [("ps", 5)], ["Pm"], scale=1.0 / 16.0)
                aux, akey = naux()
                for kt in range(2):
                    mm(aux, onesb[:], Pm[:, kt, :], kt == 0, kt == 1, ["Pm", "const"], [akey])
                recip(rsm[:], aux, [akey], ["rsm"])
                for dc in range(2):
                    pj, pkey = npj()
                    for kt in range(2):
                        mm(pj, mv[:, kt, h * 256 + dc * 128:h * 256 + (dc + 1) * 128], Pm[:, kt, :], kt == 0, kt == 1, ["mv", "Pm"], [pkey])
                    w = W[rot("W", 2)]
                    wk = id(w)
                    tt("dve", w["t2"][:], pj, rsm[:], ALU.mult, [pkey, "rsm"], [("t2", wk)])
                    o, okey = nob()
                    tt("pool", o[:], w["t2"][:], gcT[:, dc, :], ALU.mult, [("t2", wk), ("gcT", dc)], [okey])
                    r0 = 2 * 1024 + (h * 2 + dc) * 128
                    st_sp(GOT[r0:r0 + 128, tsl], o[:], [okey])
        P.barrier()

    def phase_B(l):
        sb.reset(pbase)
        Kt = [sb.alloc([128, TP], BF16) for _ in range(2)]
        Vt = [sb.alloc([128, NKT, 128], BF16) for _ in range(2)]
        Qt = [sb.alloc([128, 512], BF16) for _ in range(2)]
        Gt = [sb.alloc([128, 512], BF16) for _ in range(2)]
        Pb = [sb.alloc([128, 2, 512], BF16) for _ in range(3)]
        rs = [sb.alloc([128, 512], F32) for _ in range(2)]
        of = [sb.alloc([128, 512], F32) for _ in range(2)]
        ocp = [sb.alloc([128, 512], F32) for _ in range(2)]
        ob = [sb.alloc([128, 512], BF16) for _ in range(2)]
        if l == 0:
            zt = sb.alloc([128, 4096], BF16)
            P.add("pool", lambda e: e.memset(zt[:], 0.0), writes=["zt"])
            xs_v = xs.rearrange("(a p r) d -> a p (r d)", p=128, r=4)
            for a in range(NSLOT // 512):
                st(xs_v[a], zt[:], ["zt"])
        s1 = [sb.alloc([128, 512], BF16) for _ in range(2)]
        s2 = [sb.alloc([128, 512], BF16) for _ in range(2)]
        assert NKP % 2 == 0
        heads = []
        for g in range(2):
            for hh in range(4):
                h = g * 4 + hh
                heads.append(dict(kind="g", kv=g, kd=128, scale=128.0 ** -0.5, q=QgT[h * 128:(h + 1) * 128, :],
                                  gt=GT[h * 128:(h + 1) * 128, :], dst=GOT[h * 128:(h + 1) * 128, :], first=(hh == 0)))
        for h in range(8):
            heads.append(dict(kind="m", kv=h, kd=96, scale=96.0 ** -0.5, q=QmT[h * 96:(h + 1) * 96, :],
                              gt=GT[1024 + h * 128:1024 + (h + 1) * 128, :], dst=GOT[1024 + h * 128:1024 + (h + 1) * 128, :], first=True))
        steps = []
        for hi, hd in enumerate(heads):
            for j in range(NT):
                for kp in range(NKP):
                    steps.append((hi, j, kp))
        state = {"kv": None}

        def load_kv(hd):
            i = rot("kv", 2)
            if hd["kind"] == "g":
                g = hd["kv"]
                nsp = max(1, TP // 2048)
                for a in range(nsp):
                    sl = slice(a * (TP // nsp), (a + 1) * (TP // nsp))
                    ld(Kt[i][:, sl], KgT[g * 128:(g + 1) * 128, sl], [("K", i)])
                for a in range(nsp):
                    sl = slice(a * (NKT // nsp), (a + 1) * (NKT // nsp))
                    ld(Vt[i][:, sl, :], Vg[g, :, sl, :], [("V", i)])
            else:
                h = hd["kv"]
                nsp = max(1, TP // 2048)
                for a in range(nsp):
                    sl = slice(a * (TP // nsp), (a + 1) * (TP // nsp))
                    ld(Kt[i][0:64, sl], KnT[h * 64:(h + 1) * 64, sl], [("K", i)])
                    ld(Kt[i][64:96, sl], KrT[:, sl], [("K", i)])
                for a in range(nsp):
                    sl = slice(a * (NKT // nsp), (a + 1) * (NKT // nsp))
                    ld(Vt[i][:, sl, :], Vm[h, :, sl, :], [("V", i)])
            return i

        cur = {}

        def emit_qk(step):
            hi, j, kp = step
            hd = heads[hi]
            if j == 0 and kp == 0 and hd["first"]:
                if state.get("pre") is not None:
                    state["kv"] = state["pre"]
                    state["pre"] = None
                else:
                    state["kv"] = load_kv(hd)
            if j == NT - 1 and kp == 0 and hi + 1 < len(heads) and heads[hi + 1]["first"] and NT > 1:
                state["pre"] = load_kv(heads[hi + 1])
            kvi = state["kv"]
            kd = hd["kd"]
            if kp == 0:
                qi = rot("Q", 2)
                ld(Qt[qi][0:kd, :], hd["q"][:, j * 512:(j + 1) * 512], [("Q", qi)])
                ld(Gt[qi][:], hd["gt"][:, j * 512:(j + 1) * 512], [("G", qi)])
                cur[(hi, j)] = dict(qi=qi, kvi=kvi, ob=rot("O", 2))
            c = cur[(hi, j)]
            sbuf = rot("S", 3)
            c[("s", kp)] = sbuf
            for t in range(2):
                kt = kp * 2 + t
                mm(bank(sbuf * 2 + t), Kt[c["kvi"]][0:kd, kt * 128:(kt + 1) * 128], Qt[c["qi"]][0:kd, :], True, True,
                   [("K", c["kvi"]), ("Q", c["qi"])], [("S", sbuf)])

        def emit_pv(step):
            hi, j, kp = step
            hd = heads[hi]
            c = cur[(hi, j)]
            sbuf = c[("s", kp)]
            pi = rot("P", 3)
            act(Pb[pi][:], bank(sbuf * 2, 2).rearrange("p (k n) -> p k n", k=2), AF.Exp, [("S", sbuf), "const"], [("P", pi)],
                bias=kbias[:, kp:kp + 1], scale=hd["scale"])
            o = c["ob"]
            for t in range(2):
                kt = kp * 2 + t
                first = (kp == 0 and t == 0)
                last = (kp == NKP - 1 and t == 1)
                mm(bank(6), Vt[c["kvi"]][:, kt, :], Pb[pi][:, t, :], first, last, [("V", c["kvi"]), ("P", pi)], [("O", 0)])
            a = kp % 2

            def issue_sum(pend):
                b2_, first_, last_ = pend
                mm(bank(7), onesb[:], s2[b2_][:], first_, last_, [("s2", b2_), "const"], [("SUM", 0)])

            if a == 0 and c.get("pend") is not None:
                issue_sum(c["pend"])
                c["pend"] = None
            tt("dve", s1[a][:], Pb[pi][:, 0, :], Pb[pi][:, 1, :], ALU.add, [("P", pi)], [("s1", a)])
            if a == 1:
                b2 = rot("s2", 2)
                tt("dve", s2[b2][:], s1[0][:], s1[1][:], ALU.add, [("s1", 0), ("s1", 1)], [("s2", b2)])
                pend = (b2, kp == 1, kp == NKP - 1)
                if kp == NKP - 1:
                    issue_sum(pend)
                else:
                    c["pend"] = pend
            if kp == NKP - 1:
                cp("dve", ocp[o][:], bank(6), [("O", 0)], [("ocp", o)])
                cp("dve", rs[o][:], bank(7), [("SUM", 0)], [("rs", o)])
                recip(rs[o][:], rs[o][:], [("rs", o)], [("rs", o)])
                tt("dve", of[o][:], ocp[o][:], rs[o][:], ALU.mult, [("ocp", o), ("rs", o)], [("of", o)])
                tt("pool", ob[o][:], of[o][:], Gt[c["qi"]][:], ALU.mult, [("of", o), ("G", c["qi"])], [("ob", o)])
                st(hd["dst"][:, j * 512:(j + 1) * 512], ob[o][:], [("ob", o)])
                del cur[(hi, j)]

        emit_qk(steps[0])
        emit_qk(steps[1])
        for si in range(len(steps)):
            if si + 2 < len(steps):
                emit_qk(steps[si + 2])
            emit_pv(steps[si])
        P.barrier()

    def phase_C(l, xsrc, last):
        sb.reset(pbase)
        w_out_b = sb.alloc([128, 8, D], BF16)
        g_ffn = sb.alloc([128, D], F32)
        w_r = sb.alloc([128, 8, 72], F32)
        b_r = sb.alloc([128, 72], F32)
        mC = sb.mark()
        stg = [sb.alloc([128, 2048], F32) for _ in range(2)]
        wv = w_out_d[l].rearrange("(c p) n -> p c n", p=128)
        for c in range(8):
            load_cast(lambda c0, w, c=c: w_out_b[:, c, c0:c0 + w], lambda c0, w, c=c: wv[:, c, c0:c0 + w], D, stg, "stg", "w_out")
        ld(g_ffn[:], norm_ffn_d[l].partition_broadcast(128), ["g_ffn"])
        ld(w_r[:], w_r_d[l].rearrange("(c p) n -> p c n", p=128), ["w_r"])
        ld(b_r[:], b_r_d[l].partition_broadcast(128), ["b_r"])
        P.add("pool", lambda e: e.memset(basec[:], 0.0), writes=["basec"])
        P.barrier()
        sb.reset(mC)
        got = [sb.alloc([128, 24, 512], BF16) for _ in range(2)]
        xt = [sb.alloc([128, D], F32) for _ in range(2)]
        xn = [sb.alloc([128, D], F32) for _ in range(2)]
        h2s = [sb.alloc([128, D], F32) for _ in range(2)]
        h2b = [sb.alloc([128, D], BF16) for _ in range(2)]
        pendc = []

        def deferc(fn):
            if pendc:
                pendc.pop(0)()
            pendc.append(fn)
        h2T = sb.alloc([128, 8, 128], F32)
        junk = sb.alloc([128, D], BF16)
        msq = sb.alloc([128, 1], F32)
        rstd = sb.alloc([128, 1], F32)
        lg = sb.alloc([128, 72], F32)
        sm = {k: sb.alloc([128, 1], F32) for k in ("gmax", "ngmax", "gsum", "m1", "m2", "d21", "e21", "den", "d1", "d2")}
        ge = sb.alloc([128, 8], F32)
        ohg = sb.alloc([128, 8], F32)
        pen = sb.alloc([128, 8], F32)
        elm = sb.alloc([128, 64], F32)
        elm2 = sb.alloc([128, 64], F32)
        oh1 = sb.alloc([128, 64], F32)
        oh2 = sb.alloc([128, 64], F32)
        Asum = sb.alloc([128, 64], F32)
        Ab = sb.alloc([128, 64], BF16)
        rank = sb.alloc([128, 64], F32)
        slot = sb.alloc([128, 64], F32)
        ovf = sb.alloc([128, 64], F32)
        tmp = sb.alloc([128, 64], F32)
        BIG = 1.0e4
        BIGIDX = 1.0e6
        GOTv = GOT.rearrange("(bc p) t -> p bc t", p=128)
        for j in range(NT):
            gi = rot("got", 2)
            for b3 in range(3):
                ld(got[gi][:, b3 * 8:(b3 + 1) * 8, :], GOTv[:, b3 * 8:(b3 + 1) * 8, j * 512:(j + 1) * 512], [("got", gi)])
            for s in range(4):
                tile = j * 4 + s
                r0 = tile * 128
                xi = rot("xt", 2)
                ld(xt[xi][:], xsrc[r0:r0 + 128, :], [("xt", xi)])
                pb = rot("po", 2) * 2
                for n in range(2):
                    for bc in range(24):
                        mm(bank(pb + n), got[gi][:, bc, s * 128:(s + 1) * 128], w_out_b[:, bc % 8, n * 512:(n + 1) * 512], bc == 0, bc == 23,
                           [("got", gi), "w_out"], [("po", pb)])
                tt("dve", xn[xi][:], bank(pb, 2), xt[xi][:], ALU.add, [("po", pb), ("xt", xi)], [("xn", xi)])
                st(xmid[r0:r0 + 128, :], xn[xi][:], [("xn", xi)])
                act(junk[:], xn[xi][:], AF.Square, [("xn", xi)], ["junk", "msq"], scale=1.0 / 32.0, accum_out=msq[:])
                act(rstd[:], msq[:], AF.Sqrt, ["msq", "const"], ["rstd"], bias=epst[:, 0:1], scale=1.0)
                recip(rstd[:], rstd[:], ["rstd"], ["rstd"])
                hq = tile % 2
                h2 = h2s[hq]
                stt("dve", h2[:], xn[xi][:], rstd[:, 0:1], g_ffn[:], ALU.mult, ALU.mult, [("xn", xi), "rstd", "g_ffn"], [("h2", hq)])
                hi_ = rot("h2b", 2)
                cp("act", h2b[hi_][:], h2[:], [("h2", hq)], [("h2b", hi_)])

                def stage2(tile=tile, hi_=hi_, hq=hq, h2=h2):
                    for c in range(8):
                        tr(bank(4, 2)[:, c * 128:(c + 1) * 128], h2[:, c * 128:(c + 1) * 128], identf[:], [("h2", hq), "const"], [("ps", 4)])
                    cp("dve", h2T[:], bank(4, 2).rearrange("p (c n) -> p c n", c=8), [("ps", 4)], ["h2T"])
                    for c in range(8):
                        mm(bank(6)[:, 0:72], h2T[:, c, :], w_r[:, c, :], c == 0, c == 7, ["h2T", "w_r"], [("ps", 6)])
                    tt("dve", lg[:], bank(6)[:, 0:72], b_r[:], ALU.add, [("ps", 6), "b_r"], ["lg"])
                    rmax(sm["gmax"][:], lg[:, 0:8], ["lg"], ["gmax"])
                    ts("dve", sm["ngmax"][:], sm["gmax"][:], -1.0, None, ALU.mult, None, ["gmax"], ["ngmax"])
                    act(ge[:], lg[:, 0:8], AF.Exp, ["lg", "ngmax"], ["ge", "gsum"], bias=sm["ngmax"][:, 0:1], scale=1.0, accum_out=sm["gsum"][:])
                    ts("dve", ohg[:], lg[:, 0:8], sm["gmax"][:, 0:1], None, ALU.is_ge, None, ["lg", "gmax"], ["ohg"])
                    ts("dve", pen[:], ohg[:], -1.0, BIG, ALU.add, ALU.mult, ["ohg"], ["pen"])
                    tt("dve", elm[:].rearrange("p (g e) -> p g e", g=8), lg[:, 8:72].rearrange("p (g e) -> p g e", g=8),
                       pen[:].unsqueeze(2).to_broadcast([128, 8, 8]), ALU.add, ["lg", "pen"], ["elm"])
                    rmax(sm["m1"][:], elm[:], ["elm"], ["m1"])
                    ts("dve", oh1[:], elm[:], sm["m1"][:, 0:1], None, ALU.is_ge, None, ["elm", "m1"], ["oh1"])
                    stt("dve", elm2[:], oh1[:], -BIG, elm[:], ALU.mult, ALU.add, ["oh1", "elm"], ["elm2"])
                    rmax(sm["m2"][:], elm2[:], ["elm2"], ["m2"])
                    ts("dve", oh2[:], elm2[:], sm["m2"][:, 0:1], None, ALU.is_ge, None, ["elm2", "m2"], ["oh2"])
                    tt("dve", sm["d21"][:], sm["m2"][:], sm["m1"][:], ALU.subtract, ["m1", "m2"], ["d21"])
                    act(sm["e21"][:], sm["d21"][:], AF.Exp, ["d21"], ["e21"])
                    ts("dve", sm["den"][:], sm["e21"][:], 1.0, sm["gsum"][:, 0:1], ALU.add, ALU.mult, ["e21", "gsum"], ["den"])
                    recip(wts[:, tile, 0:1], sm["den"][:], ["den"], [("wts", tile)])
                    tt("dve", wts[:, tile, 1:2], wts[:, tile, 0:1], sm["e21"][:], ALU.mult, [("wts", tile), "e21"], [("wts", tile)])
                    tt("dve", Asum[:], oh1[:], oh2[:], ALU.add, ["oh1", "oh2"], ["Asum"])
                    ts("dve", Ab[:], Asum[:], tvalid[:, tile:tile + 1], None, ALU.mult, None, ["Asum", "const"], ["Ab"])
                    mm(bank(7)[:, 0:64], ustr[:], Ab[:], True, True, ["Ab", "const"], [("ps", 7)])
                    mm(bank(7)[:, 64:128], onesb[:], Ab[:], True, True, ["Ab", "const"], [("ps", 7)])
                    tt("dve", rank[:], bank(7)[:, 0:64], basec[:], ALU.add, [("ps", 7), "basec"], ["rank"])
                    tt("dve", basec[:], bank(7)[:, 64:128], basec[:], ALU.add, [("ps", 7), "basec"], ["basec"])
                    ts("dve", ovf[:], rank[:], float(CAP), BIGIDX, ALU.is_ge, ALU.mult, ["rank"], ["ovf"])
                    tt("dve", slot[:], rank[:], eoff[:], ALU.add, ["rank", "const"], ["slot"])
                    tt("dve", slot[:], slot[:], ovf[:], ALU.add, ["slot", "ovf"], ["slot"])
                    for k, oh, dk in ((0, oh1, "d1"), (1, oh2, "d2")):
                        tt("dve", tmp[:], oh[:], slot[:], ALU.mult, ["oh1", "oh2", "slot"], ["tmp"])
                        rsum(sm[dk][:], tmp[:], ["tmp"], [dk])
                        ts("dve", sm[dk][:], sm[dk][:], invbig[:, tile:tile + 1], None, ALU.add, None, [dk, "const"], [dk])
                        cp("dve", dest_i[:, tile, k:k + 1], sm[dk][:], [dk], [("dest", tile)])
                        P.add("pool", lambda e, k=k, tile=tile, hi_=hi_: e.indirect_dma_start(
                            out=xs, out_offset=bass.IndirectOffsetOnAxis(ap=dest_i[:, tile, k:k + 1], axis=0),
                            in_=h2b[hi_][:], in_offset=None, bounds_check=regs["bc"], oob_is_err=False),
                            reads=[("h2b", hi_), ("dest", tile)], writes=[], grp="ind")

                deferc(stage2)
        while pendc:
            pendc.pop(0)()
        P.barrier()
        chk("C1")
        sb.reset(pbase)
        CR = CAP
        wgb = [sb.alloc([128, 8, 512], BF16) for _ in range(2)]
        wdb = [sb.alloc([128, 2, D], BF16) for _ in range(2)]
        xsb = [sb.alloc([128, CB, D], BF16) for _ in range(2)]
        xbT = sb.alloc([128, 8, CR], BF16)
        sil = sb.alloc([128, 2, CR], F32)
        actT = sb.alloc([128, 2, CR], BF16)
        yo = [sb.alloc([128, D], F32) for _ in range(2)]
        def c2_loads(e_):
            wi = e_ % 2
            gv = w_gu_d[l, e_].rearrange("(c p) n -> p c n", p=128)
            for hlf in range(2):
                ld(wgb[wi][:, hlf * 4:(hlf + 1) * 4, :], gv[:, hlf * 4:(hlf + 1) * 4, :], [("wgb", wi)], eng="pool", grp="ldc")
            ld(wdb[wi][:], w_dn_d[l, e_].rearrange("(c p) n -> p c n", p=128), [("wdb", wi)], eng="pool", grp="ldc")
            ld(xsb[wi][:], xs[e_ * CAP:(e_ + 1) * CAP, :].rearrange("(b p) d -> p b d", p=128), [("xsb", wi)])

        c2_loads(0)
        for e_ in range(64):
            wi = e_ % 2
            if e_ + 1 < 64:
                c2_loads(e_ + 1)
            for b in range(CB):
                tb = rot("tb", 2)
                pT = bank(tb).bitcast(BF16)
                for c in range(8):
                    tr(pT[:, c * 128:(c + 1) * 128], xsb[wi][:, b, c * 128:(c + 1) * 128], identb[:], [("xsb", wi), "const"], [("ps", tb)])
                cp("dve", xbT[:, :, b * 128:(b + 1) * 128], pT.rearrange("p (c n) -> p c n", c=8), [("ps", tb)], ["xbT"])
            for ch in range(4):
                for c in range(8):
                    mm(bank(2 + ch)[:, 0:CR], wgb[wi][:, c, ch * 128:(ch + 1) * 128], xbT[:, c, :], c == 0, c == 7, [("wgb", wi), "xbT"], [("ps", 2 + ch)])
            for fc in range(2):
                act(sil[:, fc, :], bank(2 + fc)[:, 0:CR], AF.Silu, [("ps", 2 + fc)], [("sil", fc)])
                tt("dve", actT[:, fc, :], sil[:, fc, :], bank(4 + fc)[:, 0:CR], ALU.mult, [("sil", fc), ("ps", 4 + fc)], ["actT"])
            for b in range(CB):
                db = 6 if b % 2 == 0 else 0
                dkeys = [("ps", db), ("ps", db + 1)]
                for n in range(2):
                    for fc in range(2):
                        mm(bank(db + n), actT[:, fc, b * 128:(b + 1) * 128], wdb[wi][:, fc, n * 512:(n + 1) * 512], fc == 0, fc == 1, ["actT", ("wdb", wi)], dkeys)
                yi = rot("yo", 2)
                cp("act" if b % 2 == 0 else "dve", yo[yi][:], bank(db, 2), dkeys, [("yo", yi)])
                r0 = e_ * CAP + b * 128
                st_sp(ys[r0:r0 + 128, :], yo[yi][:], [("yo", yi)])
        P.barrier()
        chk("C2")
        sb.reset(pbase)
        y0 = [sb.alloc([128, D], F32) for _ in range(2)]
        y1 = [sb.alloc([128, D], F32) for _ in range(2)]
        xn = [sb.alloc([128, D], F32) for _ in range(2)]
        acc = [sb.alloc([128, D], F32) for _ in range(2)]
        outt = [sb.alloc([128, D], F32) for _ in range(2)]
        g_fin = sb.alloc([128, D], F32)
        junk = sb.alloc([128, D], BF16)
        msq = sb.alloc([128, 1], F32)
        rstd = sb.alloc([128, 1], F32)
        ld(g_fin[:], norm_final_d.partition_broadcast(128), ["g_fin"])
        for i in range(2):
            P.add("pool", lambda e, i=i: e.memset(y0[i][:], 0.0), writes=[("y0", i)])
            P.add("pool", lambda e, i=i: e.memset(y1[i][:], 0.0), writes=[("y1", i)])
        for tile in range(NKT):
            r0 = tile * 128
            i = rot("c3", 2)
            ld(xn[i][:], xmid[r0:r0 + 128, :], [("xn", i)])
            for k, yy, yk in ((0, y0, "y0"), (1, y1, "y1")):
                P.add("pool", lambda e, k=k, yy=yy, i=i, tile=tile: e.indirect_dma_start(
                    out=yy[i][:], out_offset=None, in_=ys,
                    in_offset=bass.IndirectOffsetOnAxis(ap=dest_i[:, tile, k:k + 1], axis=0),
                    bounds_check=regs["bc"], oob_is_err=False), reads=[], writes=[(yk, i)], grp="ind")
            stt("dve", acc[i][:], y0[i][:], wts[:, tile, 0:1], xn[i][:], ALU.mult, ALU.add, [("y0", i), ("xn", i)], [("acc", i)])
            if not last:
                stt("dve", outt[i][:], y1[i][:], wts[:, tile, 1:2], acc[i][:], ALU.mult, ALU.add, [("y1", i), ("acc", i)], [("outt", i)])
                st(xres[r0:r0 + 128, :], outt[i][:], [("outt", i)])
            else:
                stt("dve", acc[i][:], y1[i][:], wts[:, tile, 1:2], acc[i][:], ALU.mult, ALU.add, [("y1", i), ("acc", i)], [("acc", i)])
                act(junk[:], acc[i][:], AF.Square, [("acc", i)], ["junk", "msq"], scale=1.0 / 32.0, accum_out=msq[:])
                act(rstd[:], msq[:], AF.Sqrt, ["msq", "const"], ["rstd"], bias=epst[:, 0:1], scale=1.0)
                recip(rstd[:], rstd[:], ["rstd"], ["rstd"])
                stt("dve", outt[i][:], acc[i][:], rstd[:, 0:1], g_fin[:], ALU.mult, ALU.mult, [("acc", i), "rstd", "g_fin"], [("outt", i)])
                st(y_out[r0:r0 + 128, :], outt[i][:], [("outt", i)])
        P.barrier()

    xsrc = x_in
    try:
        for l in range(L):
            phase_A(l, xsrc)
            chk("A")
            phase_B(l)
            chk("B")
            phase_C(l, xsrc, last=(l == L - 1))
            xsrc = xres
    except _Stop:
        P.barrier()
    P.emit()
    return nc


def _rope_tables(TP, rot_dim):
    rows = TP // 64
    row_idx = np.repeat(np.arange(rows, dtype=np.float32), 64)
    col_idx = np.tile(np.arange(64, dtype=np.float32), rows)
    n_freq = rot_dim // 4
    inv_freq = (np.float32(10000.0) ** (-np.arange(n_freq, dtype=np.float32) / np.float32(n_freq))).astype(np.float32)
    ang = np.concatenate([row_idx[:, None] * inv_freq, col_idx[:, None] * inv_freq], axis=-1).astype(np.float32)
    return np.cos(ang).astype(np.float32), np.sin(ang).astype(np.float32)


def _rot_T(n, base, half):
    R = np.zeros((n, n), np.float32)
    for i in range(half):
        R[base + i, base + i + half] = -1.0
        R[base + i + half, base + i] = 1.0
    return np.ascontiguousarray(R.T)


def make_consts(TP, CAP):
    bf = ml_dtypes.bfloat16
    cg, sg = _rope_tables(TP, 128)
    cm, sm = _rope_tables(TP, 32)
    c = {}
    c["cgT"] = np.ascontiguousarray(np.concatenate([cg, cg], axis=1).T)
    c["sgT"] = np.ascontiguousarray(np.concatenate([sg, sg], axis=1).T)
    cmT = np.ones((96, TP), np.float32)
    smT = np.zeros((96, TP), np.float32)
    cmT[64:96] = np.concatenate([cm, cm], axis=1).T
    smT[64:96] = np.concatenate([sm, sm], axis=1).T
    c["cmT"] = cmT
    c["smT"] = smT
    c["identb"] = np.eye(128, dtype=np.float32).astype(bf)
    c["identf"] = np.eye(128, dtype=np.float32)
    c["onesb"] = np.ones((128, 128), np.float32).astype(bf)
    c["onesf"] = np.ones((128, 128), np.float32)
    c["ustr"] = np.triu(np.ones((128, 128), np.float32), 1).astype(bf)
    c["rgT"] = _rot_T(128, 0, 64)
    c["rmT"] = _rot_T(96, 64, 16)
    c["rkT"] = _rot_T(32, 0, 16)
    c["eoff"] = np.ascontiguousarray(np.broadcast_to((np.arange(64, dtype=np.float32) * CAP)[None, :], (128, 64)))
    return c


def core_inputs(x, mem, S, TP, shared):
    NKT = TP // 128
    NKP = NKT // 2
    xp = np.zeros((TP, D), np.float32)
    xp[:S] = x
    d = dict(shared)
    d["x"] = xp
    d["mem"] = np.ascontiguousarray(mem, dtype=np.float32)
    kb = np.where(np.arange(NKP) * 256 < S, 0.0, -30000.0).astype(np.float32)
    d["kbias"] = np.ascontiguousarray(np.broadcast_to(kb[None, :], (128, NKP)))
    tok = np.arange(NKT)[None, :] * 128 + np.arange(128)[:, None]
    valid = (tok < S).astype(np.float32)
    d["tvalid"] = valid
    d["invbig"] = ((1.0 - valid) * 1.0e6).astype(np.float32)
    return d


def shared_inputs(TP, CAP, w):
    s = make_consts(TP, CAP)
    f = lambda a: np.ascontiguousarray(np.asarray(a, dtype=np.float32))
    s["w_in"] = f(w["w_in"])
    s["w_qb"] = f(w["mla_w_qb"])
    kvb = f(w["mla_w_kvb"]).reshape(-1, 256, 8, 192)
    s["w_kvbk"] = np.ascontiguousarray(kvb[:, :, :, :64].reshape(-1, 256, 512))
    s["w_kvbv"] = np.ascontiguousarray(kvb[:, :, :, 64:].reshape(-1, 256, 1024))
    s["mem_w_kv"] = f(w["mem_w_kv"])
    s["w_out"] = f(w["w_out"])
    s["w_r"] = np.ascontiguousarray(np.concatenate([f(w["w_group"]), f(w["w_expert"])], axis=-1))
    s["b_r"] = np.ascontiguousarray(np.concatenate([f(w["b_group"]), f(w["b_expert"])], axis=-1))
    s["w_gu"] = f(w["w_gate_up"])
    s["w_dn"] = f(w["w_down"])
    for k in ("norm_mix", "norm_mem", "norm_ffn", "gqa_q_norm", "gqa_k_norm", "mla_q_a_norm", "mla_kv_a_norm", "norm_final"):
        s[k] = f(w[k])
    return s


def kernel(x_prompt, x_sample, mem_prompt, mem_sample, **w):
    TP = 8192
    depth = 2
    CAP = max(1, (4 * TP // 64 + 127) // 128) * 128
    x_prompt = np.asarray(x_prompt)
    x_sample = np.asarray(x_sample)
    mem_prompt = np.asarray(mem_prompt)
    mem_sample = np.asarray(mem_sample)
    shared = shared_inputs(TP, CAP, w)
    in_maps = []
    for b in range(4):
        in_maps.append(core_inputs(x_prompt[b], mem_prompt[b], 4096, TP, shared))
    for b in range(4):
        in_maps.append(core_inputs(x_sample[b], mem_sample[b], 8192, TP, shared))
    nc = build(TP, depth)
    res = run_bass_kernel_spmd(nc, in_maps, core_ids=list(range(8)))
    yp = np.stack([np.asarray(res.results[b]["y"])[:4096] for b in range(4)]).astype(np.float32)
    ysm = np.stack([np.asarray(res.results[4 + b]["y"]) for b in range(4)]).astype(np.float32)
    return (yp, ysm)
```

```python
import contextlib
import numpy as np
import ml_dtypes
import concourse.bass as bass
import concourse.mybir as mybir
from concourse.bass_utils import run_bass_kernel_spmd

F32 = mybir.dt.float32
BF16 = mybir.dt.bfloat16
I32 = mybir.dt.int32
AF = mybir.ActivationFunctionType
ALU = mybir.AluOpType
AX = mybir.AxisListType

D = 1024
IN_W = 6304
C_QG, C_KG, C_VG, C_QA, C_KVA, C_KR, C_QC, C_GATE = 0, 1024, 1280, 1536, 1920, 2176, 2208, 3232
EPS = 1e-6
SBUF_BASE = 16512
SBUF_END = 229376


class Op:
    __slots__ = ("eng", "fn", "deps", "signal", "count", "grp", "gidx")


class Prog:
    ENGS = ("pe", "act", "dve", "pool", "sp")

    def __init__(self, nc):
        self.nc = nc
        self.ops = {e: [] for e in self.ENGS}
        self.last_w = {}
        self.readers = {}
        self.groups = {}

    def dma_group(self, name, n):
        self.groups[name] = dict(n=n, ops=[], sems=None)

    def _mk(self, eng, fn, grp):
        op = Op()
        op.eng = eng
        op.fn = fn
        op.signal = False
        op.count = None
        op.grp = grp
        op.gidx = None
        return op

    PSK = ("ps", "S", "O", "SUM", "po")

    def add(self, eng, fn, reads=(), writes=(), grp=None):
        pk = [k for k in reads if isinstance(k, tuple) and k[0] in self.PSK]
        if pk:
            reads = [k for k in reads if k not in pk]
            writes = list(writes) + pk
        op = self._mk(eng, fn, grp)
        deps = set()
        for k in reads:
            w = self.last_w.get(k)
            if w is not None:
                deps.add(w)
        for k in writes:
            w = self.last_w.get(k)
            if w is not None:
                deps.add(w)
            rd = self.readers.get(k)
            if rd:
                for r in rd.values():
                    if isinstance(r, list):
                        deps.update(r)
                    else:
                        deps.add(r)
        if grp is not None:
            g = self.groups[grp]
            i = len(g["ops"])
            op.gidx = i
            if i >= g["n"]:
                deps.add(g["ops"][i - g["n"]])
            g["ops"].append(op)
        if eng == "pe" and grp is None:
            deps = {d for d in deps if not (d.eng == "pe" and d.grp is None)}
        for d in deps:
            if d.grp is None:
                d.signal = True
        op.deps = deps
        for k in reads:
            rd = self.readers.setdefault(k, {})
            if grp is not None:
                rd.setdefault("dma", []).append(op)
            else:
                rd[eng] = op
        for k in writes:
            self.last_w[k] = op
            self.readers[k] = {}
        self.ops[eng].append(op)
        return op

    def barrier(self):
        lasts = []
        for e in self.ENGS:
            for op in reversed(self.ops[e]):
                if op.grp is None and op.fn is not None:
                    lasts.append(op)
                    break
        for g in self.groups.values():
            lasts.extend(g["ops"][-g["n"]:])
        for d in lasts:
            if d.grp is None:
                d.signal = True
        for e in self.ENGS:
            op = self._mk(e, None, None)
            op.deps = set(lasts)
            self.ops[e].append(op)
        self.last_w = {}
        self.readers = {}

    def wait_all(self, eng, ops):
        op = self._mk(eng, None, None)
        op.deps = set(ops)
        for d in ops:
            if d.grp is None:
                d.signal = True
        self.ops[eng].append(op)

    def emit(self):
        nc = self.nc
        for e in self.ENGS:
            c = 0
            for op in self.ops[e]:
                if op.grp is None and op.signal:
                    c += 1
                    op.count = c
        with contextlib.ExitStack() as st:
            esem = {e: st.enter_context(nc.semaphore("s_" + e)) for e in self.ENGS}
            for gname, g in self.groups.items():
                g["sems"] = [st.enter_context(nc.semaphore("g_%s_%d" % (gname, i))) for i in range(g["n"])]
            block = st.enter_context(nc.Block())
            prog = self

            def semval(d):
                if d.grp is not None:
                    g = prog.groups[d.grp]
                    return g["sems"][d.gidx % g["n"]], 16 * (d.gidx // g["n"] + 1)
                return esem[d.eng], d.count

            def run(ename, eng):
                known = {}
                init = getattr(prog, "init_" + ename, None)
                if init is not None:
                    init(eng)
                for op in prog.ops[ename]:
                    waits = {}
                    for d in op.deps:
                        s, v = semval(d)
                        key = id(s)
                        if key not in waits or waits[key][1] < v:
                            waits[key] = (s, v)
                    for key, (s, v) in waits.items():
                        if known.get(key, 0) >= v:
                            continue
                        eng.wait_ge(s, v)
                        known[key] = v
                    if op.fn is None:
                        continue
                    inst = op.fn(eng)
                    if op.grp is not None:
                        s, v = semval(op)
                        inst.then_inc(s, 16)
                    elif op.signal:
                        inst.then_inc(esem[ename], 1)

            @block.tensor
            def _(e):
                run("pe", e)

            @block.scalar
            def _(e):
                run("act", e)

            @block.vector
            def _(e):
                run("dve", e)

            @block.gpsimd
            def _(e):
                run("pool", e)

            @block.sync
            def _(e):
                run("sp", e)


class SB:
    def __init__(self, nc):
        self.nc = nc
        self.off = SBUF_BASE
        self.n = 0

    def alloc(self, shape, dt):
        esz = 4 if dt in (F32, I32) else 2
        sz = int(np.prod(shape[1:])) * esz
        sz = (sz + 63) // 64 * 64
        assert self.off + sz <= SBUF_END, ("SBUF overflow", self.off, sz)
        self.n += 1
        t = self.nc.alloc_sbuf_tensor_at("t%d" % self.n, list(shape), dt, offset=self.off)
        self.off += sz
        return t

    def mark(self):
        return self.off

    def reset(self, m):
        self.off = m


class _Stop(Exception):
    pass


def build(TP, depth, dbg=False, stop=None):
    nc = bass.Bass("TRN2", target_bir_lowering=False)
    NT = TP // 512
    NKT = TP // 128
    NKP = NKT // 2
    CB = max(1, (4 * TP // 64 + 127) // 128)
    CAP = CB * 128
    NSLOT = 64 * CAP
    L = depth

    def din(name, shape, dt=F32):
        return nc.dram_tensor(name, list(shape), dt, kind="ExternalInput").ap()

    def dscr(name, shape, dt):
        return nc.dram_tensor(name, list(shape), dt, kind=("ExternalOutput" if dbg else "Internal")).ap()

    x_in = din("x", [TP, D])
    mem_in = din("mem", [256, D])
    kbias_in = din("kbias", [128, NKP])
    tvalid_in = din("tvalid", [128, NKT])
    invbig_in = din("invbig", [128, NKT])
    eoff_in = din("eoff", [128, 64])
    cgT_in = din("cgT", [128, TP])
    sgT_in = din("sgT", [128, TP])
    cmT_in = din("cmT", [96, TP])
    smT_in = din("smT", [96, TP])
    identb_in = din("identb", [128, 128], BF16)
    identf_in = din("identf", [128, 128])
    onesb_in = din("onesb", [128, 128], BF16)
    onesf_in = din("onesf", [128, 128])
    ustr_in = din("ustr", [128, 128], BF16)
    rgT_in = din("rgT", [128, 128])
    rmT_in = din("rmT", [96, 96])
    rkT_in = din("rkT", [32, 32])
    w_in_d = din("w_in", [L, D, IN_W])
    w_qb_d = din("w_qb", [L, 384, 768])
    w_kvbk_d = din("w_kvbk", [L, 256, 512])
    w_kvbv_d = din("w_kvbv", [L, 256, 1024])
    memw_d = din("mem_w_kv", [L, D, 2048])
    w_out_d = din("w_out", [L, D, D])
    w_r_d = din("w_r", [L, D, 72])
    b_r_d = din("b_r", [L, 72])
    w_gu_d = din("w_gu", [L, 64, D, 512])
    w_dn_d = din("w_dn", [L, 64, 256, D])
    norm_mix_d = din("norm_mix", [L, D])
    norm_mem_d = din("norm_mem", [L, D])
    norm_ffn_d = din("norm_ffn", [L, D])
    gq_d = din("gqa_q_norm", [L, 128])
    gk_d = din("gqa_k_norm", [L, 128])
    gqa_d = din("mla_q_a_norm", [L, 384])
    gkva_d = din("mla_kv_a_norm", [L, 256])
    norm_final_d = din("norm_final", [D])
    y_out = nc.dram_tensor("y", [TP, D], F32, kind="ExternalOutput").ap()

    QgT = dscr("QgT", [8 * 128, TP], BF16)
    KgT = dscr("KgT", [2 * 128, TP], BF16)
    Vg = dscr("Vg", [2, 128, NKT, 128], BF16)
    QmT = dscr("QmT", [8 * 96, TP], BF16)
    KnT = dscr("KnT", [8 * 64, TP], BF16)
    KrT = dscr("KrT", [32, TP], BF16)
    Vm = dscr("Vm", [8, 128, NKT, 128], BF16)
    GT = dscr("GT", [2 * D, TP], BF16)
    GOT = dscr("GOT", [3 * D, TP], BF16)
    xmid = dscr("xmid", [TP, D], F32)
    xres = dscr("xres", [TP, D], F32)
    xs = dscr("xs", [NSLOT, D], BF16)
    ys = dscr("ys", [NSLOT, D], F32)

    P = Prog(nc)
    regs = {}

    def pool_init(eng):
        regs["bc"] = eng.alloc_register("bc")
        eng.reg_mov(regs["bc"], NSLOT - 1)

    P.init_pool = pool_init
    P.dma_group("ld", 8)
    P.dma_group("ld2", 4)
    P.dma_group("st", 8)
    P.dma_group("ind", 8)
    P.dma_group("stsp", 8)
    P.dma_group("lda", 6)
    P.dma_group("ldc", 12)
    sb = SB(nc)
    ps = nc.alloc_psum_tensor("ps", [128, 4096], F32)

    def chk(name):
        if stop == name:
            raise _Stop()

    def bank(i, n=1):
        return ps[:, i * 512:(i + n) * 512]

    cnt = {}

    def rot(name, n):
        c = cnt.get(name, 0)
        cnt[name] = c + 1
        return c % n

    def ld(out, in_, writes, reads=(), grp="ld", eng="sp", **kw):
        return P.add(eng, lambda e: e.dma_start(out=out, in_=in_, **kw), reads=reads, writes=writes, grp=grp)

    def st(out, in_, reads, writes=(), eng="pool", grp="st", **kw):
        return P.add(eng, lambda e: e.dma_start(out=out, in_=in_, **kw), reads=reads, writes=writes, grp=grp)

    def st_sp(out, in_, reads, writes=()):
        return st(out, in_, reads, writes, eng="sp", grp="stsp")

    def mm(out, lhsT, rhs, start, stop, reads, writes):
        return P.add("pe", lambda e: e.matmul(out, lhsT=lhsT, rhs=rhs, start=start, stop=stop), reads=reads, writes=writes)

    def tr(out, in_, ident, reads, writes):
        return P.add("pe", lambda e: e.transpose(out=out, in_=in_, identity=ident), reads=reads, writes=writes)

    def act(out, in_, func, reads, writes, **kw):
        return P.add("act", lambda e: e.activation(out=out, in_=in_, func=func, **kw), reads=reads, writes=writes)

    def tt(eng, out, in0, in1, op, reads, writes):
        return P.add(eng, lambda e: e.tensor_tensor(out=out, in0=in0, in1=in1, op=op), reads=reads, writes=writes)

    def ts(eng, out, in0, s1, s2, op0, op1, reads, writes):
        if op1 is None:
            return P.add(eng, lambda e: e.tensor_scalar(out=out, in0=in0, scalar1=s1, scalar2=None, op0=op0), reads=reads, writes=writes)
        return P.add(eng, lambda e: e.tensor_scalar(out=out, in0=in0, scalar1=s1, scalar2=s2, op0=op0, op1=op1), reads=reads, writes=writes)

    def stt(eng, out, in0, scalar, in1, op0, op1, reads, writes):
        return P.add(eng, lambda e: e.scalar_tensor_tensor(out=out, in0=in0, scalar=scalar, in1=in1, op0=op0, op1=op1), reads=reads, writes=writes)

    def cp(eng, out, in_, reads, writes):
        if eng == "act":
            return act(out, in_, AF.Copy, reads, writes)
        return P.add(eng, lambda e: e.tensor_copy(out=out, in_=in_), reads=reads, writes=writes)

    def recip(out, in_, reads, writes):
        return P.add("dve", lambda e: e.reciprocal(out=out, in_=in_), reads=reads, writes=writes)

    def rmax(out, in_, reads, writes):
        return P.add("dve", lambda e: e.reduce_max(out=out, in_=in_, axis=AX.X), reads=reads, writes=writes)

    def rsum(out, in_, reads, writes):
        return P.add("dve", lambda e: e.reduce_sum(out=out, in_=in_, axis=AX.X), reads=reads, writes=writes)

    identb = sb.alloc([128, 128], BF16)
    identf = sb.alloc([128, 128], F32)
    onesb = sb.alloc([128, 128], BF16)
    onesf = sb.alloc([128, 128], F32)
    ustr = sb.alloc([128, 128], BF16)
    rgT = sb.alloc([128, 128], F32)
    rmT = sb.alloc([128, 96], F32)
    rkT = sb.alloc([128, 32], F32)
    kbias = sb.alloc([128, NKP], F32)
    tvalid = sb.alloc([128, NKT], F32)
    invbig = sb.alloc([128, NKT], F32)
    eoff = sb.alloc([128, 64], F32)
    epst = sb.alloc([128, 1], F32)
    dest_i = sb.alloc([128, NKT, 2], I32)
    wts = sb.alloc([128, NKT, 2], F32)
    basec = sb.alloc([128, 64], F32)
    for t, src in ((identb, identb_in), (identf, identf_in), (onesb, onesb_in), (onesf, onesf_in), (ustr, ustr_in),
                   (rgT, rgT_in), (kbias, kbias_in), (tvalid, tvalid_in), (invbig, invbig_in), (eoff, eoff_in)):
        ld(t[:], src, ["const"])
    ld(rmT[0:96, :], rmT_in, ["const"])
    ld(rkT[0:32, :], rkT_in, ["const"])
    P.add("pool", lambda e: e.memset(epst[:], EPS), writes=["const"])

    pbase = sb.mark()
    chk("init")

    def load_cast(dst_fn, src_fn, ncols, stg, key, wkey):
        c0 = 0
        while c0 < ncols:
            w = min(2048, ncols - c0)
            ld(dst_fn(c0, w), src_fn(c0, w), [wkey], eng="pool", grp="ldc")
            c0 += w

    def rms_token_major(xt, xkey, hb, hbkey, msq, rstd, junk):
        act(junk[:], xt, AF.Square, [xkey], ["junk", "msq"], scale=1.0 / 32.0, accum_out=msq[:])
        act(rstd[:], msq[:], AF.Ln, ["msq", "const"], ["rstd"], bias=epst[:, 0:1], scale=1.0)
        act(rstd[:], rstd[:], AF.Exp, ["rstd"], ["rstd"], scale=-0.5)
        act(hb, xt, AF.Copy, [xkey, "rstd"], [hbkey], scale=rstd[:, 0:1])

    def phase_A(l, xsrc):
        sb.reset(pbase)
        g_mix = sb.alloc([128, 8], F32)
        g_mem = sb.alloc([128, 8], F32)
        g_q = sb.alloc([128, 1], F32)
        g_k = sb.alloc([128, 1], F32)
        g_qa = sb.alloc([128, 3], F32)
        g_kva = sb.alloc([128, 2], F32)
        ld(g_mix[:], norm_mix_d[l].rearrange("(c p) -> p c", p=128), ["gains"], allow_slow_non_contiguous=True)
        ld(g_mem[:], norm_mem_d[l].rearrange("(c p) -> p c", p=128), ["gains"], allow_slow_non_contiguous=True)
        ld(g_q[:], gq_d[l].rearrange("(c p) -> p c", p=128), ["gains"], allow_slow_non_contiguous=True)
        ld(g_k[:], gk_d[l].rearrange("(c p) -> p c", p=128), ["gains"], allow_slow_non_contiguous=True)
        ld(g_qa[:], gqa_d[l].rearrange("(c p) -> p c", p=128), ["gains"], allow_slow_non_contiguous=True)
        ld(g_kva[:], gkva_d[l].rearrange("(c p) -> p c", p=128), ["gains"], allow_slow_non_contiguous=True)
        mkT = sb.alloc([128, 8, 256], BF16)
        mv = sb.alloc([128, 2, 1024], BF16)
        w_qb_b = sb.alloc([128, 3, 768], BF16)
        w_kvbk_b = sb.alloc([128, 2, 512], BF16)
        w_kvbv_b = sb.alloc([128, 2, 1024], BF16)
        msq = sb.alloc([128, 1], F32)
        rstd = sb.alloc([128, 1], F32)
        junk = sb.alloc([128, 1024], BF16)
        psT = bank(0).bitcast(BF16)
        mA = sb.mark()
        stg = [sb.alloc([128, 2048], F32) for _ in range(2)]
        memw_b = sb.alloc([128, 8, 2048], BF16)
        memt = sb.alloc([128, 2, 1024], F32)
        membf = sb.alloc([128, 1024], BF16)
        memT = sb.alloc([128, 8, 256], BF16)
        mwv = memw_d[l].rearrange("(c p) n -> p c n", p=128)
        for c in range(8):
            load_cast(lambda c0, w, c=c: memw_b[:, c, c0:c0 + w], lambda c0, w, c=c: mwv[:, c, c0:c0 + w], 2048, stg, "stg", "memw")
        ld(memt[:], mem_in.rearrange("(kt p) d -> p kt d", p=128), ["memt"])
        for kt in range(2):
            rms_token_major(memt[:, kt, :], "memt", membf[:], "membf", msq, rstd, junk)
            for c in range(8):
                tr(psT[:, c * 128:(c + 1) * 128], membf[:, c * 128:(c + 1) * 128], identb[:], ["membf", "const"], [("ps", 0)])
            tt("dve", memT[:, :, kt * 128:(kt + 1) * 128], psT.rearrange("p (c n) -> p c n", c=8),
               g_mem[:].unsqueeze(2).to_broadcast([128, 8, 128]), ALU.mult, [("ps", 0), "gains"], ["memT"])
        for ch in range(8):
            b = 1 + rot("pj", 2)
            for kc in range(8):
                mm(bank(b)[:, 0:256], memw_b[:, kc, ch * 128:(ch + 1) * 128], memT[:, kc, :], kc == 0, kc == 7, ["memw", "memT"], [("ps", b)])
            cp("act", mkT[:, ch, :], bank(b)[:, 0:256], [("ps", b)], ["mkT"])
        for kt in range(2):
            for n in range(2):
                b = 1 + rot("pj", 2)
                for kc in range(8):
                    mm(bank(b), memT[:, kc, kt * 128:(kt + 1) * 128], memw_b[:, kc, 1024 + n * 512:1024 + (n + 1) * 512], kc == 0, kc == 7, ["memw", "memT"], [("ps", b)])
                cp("act", mv[:, kt, n * 512:(n + 1) * 512], bank(b), [("ps", b)], ["mv"])
        P.barrier()
        chk("A_mem")
        sb.reset(mA)
        w_in_b = sb.alloc([128, 8, IN_W], BF16)
        mW = sb.mark()
        stg = [sb.alloc([128, 2048], F32) for _ in range(2)]
        wv = w_in_d[l].rearrange("(c p) n -> p c n", p=128)
        for c in range(8):
            load_cast(lambda c0, w, c=c: w_in_b[:, c, c0:c0 + w], lambda c0, w, c=c: wv[:, c, c0:c0 + w], IN_W, stg, "stg", "w_in")
        v = w_qb_d[l].rearrange("(c p) n -> p c n", p=128)
        for c in range(3):
            load_cast(lambda c0, w, c=c: w_qb_b[:, c, c0:c0 + w], lambda c0, w, c=c: v[:, c, c0:c0 + w], 768, stg, "stg", "w_qb")
        v2 = w_kvbk_d[l].rearrange("(c p) n -> p c n", p=128)
        v3 = w_kvbv_d[l].rearrange("(c p) n -> p c n", p=128)
        for c in range(2):
            load_cast(lambda c0, w, c=c: w_kvbk_b[:, c, c0:c0 + w], lambda c0, w, c=c: v2[:, c, c0:c0 + w], 512, stg, "stg", "w_kvbk")
            load_cast(lambda c0, w, c=c: w_kvbv_b[:, c, c0:c0 + w], lambda c0, w, c=c: v3[:, c, c0:c0 + w], 1024, stg, "stg", "w_kvbv")
        P.barrier()
        chk("A_w")
        sb.reset(mW)
        xt = [sb.alloc([128, 1024], F32) for _ in range(2)]
        hb = [sb.alloc([128, 1024], BF16) for _ in range(2)]
        hT = sb.alloc([128, 8, 512], BF16)
        Cg = sb.alloc([128, 512], F32)
        Sg = sb.alloc([128, 512], F32)
        Cm = sb.alloc([128, 512], F32)
        Sm = sb.alloc([128, 512], F32)
        Ck = sb.alloc([128, 512], F32)
        Sk = sb.alloc([128, 512], F32)
        W = [dict(qsq=sb.alloc([128, 512], F32), rs=sb.alloc([128, 512], F32), qn=sb.alloc([128, 512], F32),
                  t1=sb.alloc([128, 512], F32), t2=sb.alloc([128, 512], F32)) for _ in range(2)]
        ob = [sb.alloc([128, 512], BF16) for _ in range(4)]
        qaf = sb.alloc([128, 3, 512], F32)
        qanT = sb.alloc([128, 3, 512], BF16)
        kvanT = sb.alloc([128, 2, 512], BF16)
        qcT = sb.alloc([128, 2, 512], BF16)
        gcT = sb.alloc([128, 2, 512], BF16)
        Pm = sb.alloc([128, 2, 512], BF16)
        rsm = sb.alloc([128, 512], F32)
        vst = [sb.alloc([128, 1024], BF16) for _ in range(2)]

        pend = []

        def defer(fn):
            if pend:
                pend.pop(0)()
            pend.append(fn)

        def flush():
            while pend:
                pend.pop(0)()

        def nob():
            i = rot("ob", 4)
            return ob[i], ("ob", i)

        def npj():
            b = 1 + rot("pj", 2)
            return bank(b), ("ps", b)

        def naux():
            b = 3 + rot("aux", 2)
            return bank(b), ("ps", b)

        def rope_out(src_f, srckey, np_, RT, Ctab, Stab, w, dst):
            aux, akey = bank(7), ("ps", 7)
            mm(aux[0:np_, :], RT, src_f, True, True, [srckey, "const"], [akey])
            tt("pool", w["t1"][0:np_, :], src_f, Ctab[0:np_, :], ALU.mult, [srckey, "tab"], [("t1", id(w))])
            tt("dve", w["t2"][0:np_, :], aux[0:np_, :], Stab[0:np_, :], ALU.mult, [akey, "tab"], [("t2", id(w))])
            o, okey = nob()
            tt("dve", o[0:np_, :], w["t1"][0:np_, :], w["t2"][0:np_, :], ALU.add, [("t1", id(w)), ("t2", id(w))], [okey])
            st_sp(dst, o[0:np_, :], [okey])

        for j in range(NT):
            t0 = j * 512
            tsl = slice(t0, t0 + 512)
            ld(Cg[:], cgT_in[:, tsl], ["tab"], grp="lda", eng="act")
            ld(Sg[:], sgT_in[:, tsl], ["tab"], grp="lda", eng="act")
            ld(Cm[0:96, :], cmT_in[:, tsl], ["tab"], grp="lda", eng="act")
            ld(Sm[0:96, :], smT_in[:, tsl], ["tab"], grp="lda", eng="act")
            ld(Ck[0:32, :], cmT_in[64:96, tsl], ["tab"], grp="lda", eng="act")
            ld(Sk[0:32, :], smT_in[64:96, tsl], ["tab"], grp="lda", eng="act")
            for s in range(4):
                i = rot("xt", 2)
                ld(xt[i][:], xsrc[t0 + s * 128:t0 + (s + 1) * 128, :], [("xt", i)], grp="lda", eng="act")
                rms_token_major(xt[i][:], ("xt", i), hb[i][:], ("hb", i), msq, rstd, junk)
                for c in range(8):
                    tr(psT[:, c * 128:(c + 1) * 128], hb[i][:, c * 128:(c + 1) * 128], identb[:], [("hb", i), "const"], [("ps", 0)])
                tt("dve", hT[:, :, s * 128:(s + 1) * 128], psT.rearrange("p (c n) -> p c n", c=8),
                   g_mix[:].unsqueeze(2).to_broadcast([128, 8, 128]), ALU.mult, [("ps", 0), "gains"], ["hT"])
            chk("A0")
            for hc in range(10):
                c0 = C_QG + hc * 128 if hc < 8 else C_KG + (hc - 8) * 128
                gain = g_q if hc < 8 else g_k
                dst = QgT[hc * 128:(hc + 1) * 128, tsl] if hc < 8 else KgT[(hc - 8) * 128:(hc - 7) * 128, tsl]
                w = W[rot("W", 2)]
                wk = id(w)
                pj, pkey = npj()
                for kc in range(8):
                    mm(pj, w_in_b[:, kc, c0:c0 + 128], hT[:, kc, :], kc == 0, kc == 7, ["w_in", "hT"], [pkey])
                act(w["qsq"][:], pj, AF.Square, [pkey], [("qsq", wk)])
                aux, akey = naux()
                mm(aux, onesf[:], w["qsq"][:], True, True, [("qsq", wk), "const"], [akey])

                def stage2(w=w, wk=wk, pj=pj, pkey=pkey, aux=aux, akey=akey, gain=gain, dst=dst):
                    act(w["rs"][:], aux, AF.Ln, [akey, "const"], [("rs", wk)], bias=epst[:, 0:1], scale=1.0 / 128.0)
                    act(w["rs"][:], w["rs"][:], AF.Exp, [("rs", wk)], [("rs", wk)], scale=-0.5)
                    stt("dve", w["qn"][:], pj, gain[:, 0:1], w["rs"][:], ALU.mult, ALU.mult, [pkey, ("rs", wk), "gains"], [("qn", wk)])
                    rope_out(w["qn"][:], ("qn", wk), 128, rgT[:], Cg, Sg, w, dst)

                defer(stage2)
            flush()
            chk("A1")
            for s in range(4):
                kt = j * 4 + s
                for kc in range(8):
                    mm(bank(7)[:, 0:256], hT[:, kc, s * 128:(s + 1) * 128], w_in_b[:, kc, C_VG:C_VG + 256], kc == 0, kc == 7, ["w_in", "hT"], [("ps", 7)])
                i = rot("vst", 2)
                cp("act", vst[i][:, 0:256], bank(7)[:, 0:256], [("ps", 7)], [("vst", i)])
                st_sp(Vg[:, :, kt, :].rearrange("g p d -> p g d"), vst[i][:, 0:256].rearrange("p (g d) -> p g d", g=2), [("vst", i)])
            chk("A2")
            for (cbase, nch, gain, dstT, dkey, dim) in ((C_QA, 3, g_qa, qanT, "qanT", 384.0), (C_KVA, 2, g_kva, kvanT, "kvanT", 256.0)):
                w = W[rot("W", 2)]
                wk = id(w)
                aux, akey = naux()
                for c in range(nch):
                    pj, pkey = npj()
                    for kc in range(8):
                        mm(pj, w_in_b[:, kc, cbase + c * 128:cbase + (c + 1) * 128], hT[:, kc, :], kc == 0, kc == 7, ["w_in", "hT"], [pkey])
                    cp("dve", qaf[:, c, :], pj, [pkey], [("qaf", c)])
                    sqn = ("qsq", "t1", "t2")[c]
                    act(w[sqn][:], pj, AF.Square, [pkey], [(sqn, wk)])
                for c in range(nch):
                    sqn = ("qsq", "t1", "t2")[c]
                    mm(aux, onesf[:], w[sqn][:], c == 0, c == nch - 1, [(sqn, wk), "const"], [akey])
                act(w["rs"][:], aux, AF.Ln, [akey, "const"], [("rs", wk)], bias=epst[:, 0:1], scale=1.0 / dim)
                act(w["rs"][:], w["rs"][:], AF.Exp, [("rs", wk)], [("rs", wk)], scale=-0.5)
                for c in range(nch):
                    stt("dve", dstT[:, c, :], qaf[:, c, :], gain[:, c:c + 1], w["rs"][:], ALU.mult, ALU.mult, [("qaf", c), ("rs", wk), "gains"], [dkey])
            chk("A4")
            w = W[rot("W", 2)]
            wk = id(w)
            pj, pkey = npj()
            for kc in range(8):
                mm(pj[0:32, :], w_in_b[:, kc, C_KR:C_KR + 32], hT[:, kc, :], kc == 0, kc == 7, ["w_in", "hT"], [pkey])
            cp("dve", w["qn"][0:32, :], pj[0:32, :], [pkey], [("qn", wk)])
            rope_out(w["qn"][0:32, :], ("qn", wk), 32, rkT[0:32, :], Ck, Sk, w, KrT[:, tsl])
            chk("A5")
            for h in range(8):
                w = W[rot("W", 2)]
                wk = id(w)
                pj, pkey = npj()
                for c in range(3):
                    mm(pj[0:96, :], w_qb_b[:, c, h * 96:(h + 1) * 96], qanT[:, c, :], c == 0, c == 2, ["w_qb", "qanT"], [pkey])
                cp("dve", w["qn"][0:96, :], pj[0:96, :], [pkey], [("qn", wk)])

                def stage2(w=w, wk=wk, h=h):
                    rope_out(w["qn"][0:96, :], ("qn", wk), 96, rmT[0:96, :], Cm, Sm, w, QmT[h * 96:(h + 1) * 96, tsl])

                defer(stage2)
            flush()
            chk("A6")
            for hp in range(4):
                pj, pkey = npj()
                for c in range(2):
                    mm(pj, w_kvbk_b[:, c, hp * 128:(hp + 1) * 128], kvanT[:, c, :], c == 0, c == 1, ["w_kvbk", "kvanT"], [pkey])
                o, okey = nob()
                cp("act", o[:], pj, [pkey], [okey])
                st_sp(KnT[hp * 128:(hp + 1) * 128, tsl], o[:], [okey])
            chk("A7")
            for s in range(4):
                kt = j * 4 + s
                i = rot("vst", 2)
                for n in range(2):
                    for c in range(2):
                        mm(bank(7), kvanT[:, c, s * 128:(s + 1) * 128], w_kvbv_b[:, c, n * 512:(n + 1) * 512], c == 0, c == 1, ["w_kvbv", "kvanT"], [("ps", 7)])
                    cp("act", vst[i][:, n * 512:(n + 1) * 512], bank(7), [("ps", 7)], [("vst", i)])
                st_sp(Vm[:, :, kt, :].rearrange("h p d -> p h d"), vst[i][:].rearrange("p (h d) -> p h d", h=8), [("vst", i)])
            chk("A8")
            for br in range(2):
                for c in range(8):
                    c0 = C_GATE + br * 1024 + c * 128
                    pj, pkey = npj()
                    for kc in range(8):
                        mm(pj, w_in_b[:, kc, c0:c0 + 128], hT[:, kc, :], kc == 0, kc == 7, ["w_in", "hT"], [pkey])
                    o, okey = nob()
                    act(o[:], pj, AF.Sigmoid, [pkey], [okey])
                    st_sp(GT[br * 1024 + c * 128:br * 1024 + (c + 1) * 128, tsl], o[:], [okey])
            chk("A9")
            for h in range(4):
                for dc in range(2):
                    c0 = C_QC + h * 256 + dc * 128
                    pj, pkey = npj()
                    for kc in range(8):
                        mm(pj, w_in_b[:, kc, c0:c0 + 128], hT[:, kc, :], kc == 0, kc == 7, ["w_in", "hT"], [pkey])
                    cp("dve", qcT[:, dc, :], pj, [pkey], [("qcT", dc)])
                for dc in range(2):
                    c0 = C_GATE + 2048 + h * 256 + dc * 128
                    pj, pkey = npj()
                    for kc in range(8):
                        mm(pj, w_in_b[:, kc, c0:c0 + 128], hT[:, kc, :], kc == 0, kc == 7, ["w_in", "hT"], [pkey])
                    act(gcT[:, dc, :], pj, AF.Sigmoid, [pkey], [("gcT", dc)])
                for kt in range(2):
                    for dc in range(2):
                        mm(bank(5 + kt), mkT[:, h * 2 + dc, kt * 128:(kt + 1) * 128], qcT[:, dc, :], dc == 0, dc == 1, ["mkT", ("qcT", dc)], [("ps", 5)])
                act(Pm[:], bank(5, 2).rearrange("p (k n) -> p k n", k=2), AF.Exp, [("ps", 5)], ["Pm"], scale=1.0 / 16.0)
                aux, akey = naux()
                for kt in range(2):
                    mm(aux, onesb[:], Pm[:, kt, :], kt == 0, kt == 1, ["Pm", "const"], [akey])
                recip(rsm[:], aux, [akey], ["rsm"])
                for dc in range(2):
                    pj, pkey = npj()
                    for kt in range(2):
                        mm(pj, mv[:, kt, h * 256 + dc * 128:h * 256 + (dc + 1) * 128], Pm[:, kt, :], kt == 0, kt == 1, ["mv", "Pm"], [pkey])
                    w = W[rot("W", 2)]
                    wk = id(w)
                    tt("dve", w["t2"][:], pj, rsm[:], ALU.mult, [pkey, "rsm"], [("t2", wk)])
                    o, okey = nob()
                    tt("pool", o[:], w["t2"][:], gcT[:, dc, :], ALU.mult, [("t2", wk), ("gcT", dc)], [okey])
                    r0 = 2 * 1024 + (h * 2 + dc) * 128
                    st_sp(GOT[r0:r0 + 128, tsl], o[:], [okey])
        P.barrier()

    def phase_B(l):
        sb.reset(pbase)
        Kt = [sb.alloc([128, TP], BF16) for _ in range(2)]
        Vt = [sb.alloc([128, NKT, 128], BF16) for _ in range(2)]
        Qt = [sb.alloc([128, 512], BF16) for _ in range(2)]
        Gt = [sb.alloc([128, 512], BF16) for _ in range(2)]
        Pb = [sb.alloc([128, 2, 512], BF16) for _ in range(3)]
        rs = [sb.alloc([128, 512], F32) for _ in range(2)]
        of = [sb.alloc([128, 512], F32) for _ in range(2)]
        ob = [sb.alloc([128, 512], BF16) for _ in range(2)]
        if l == 0:
            zt = sb.alloc([128, 4096], BF16)
            P.add("pool", lambda e: e.memset(zt[:], 0.0), writes=["zt"])
            xs_v = xs.rearrange("(a p r) d -> a p (r d)", p=128, r=4)
            for a in range(NSLOT // 512):
                st(xs_v[a], zt[:], ["zt"])
        s1 = [sb.alloc([128, 512], BF16) for _ in range(2)]
        s2 = [sb.alloc([128, 512], BF16) for _ in range(2)]
        assert NKP % 2 == 0
        heads = []
        for g in range(2):
            for hh in range(4):
                h = g * 4 + hh
                heads.append(dict(kind="g", kv=g, kd=128, scale=128.0 ** -0.5, q=QgT[h * 128:(h + 1) * 128, :],
                                  gt=GT[h * 128:(h + 1) * 128, :], dst=GOT[h * 128:(h + 1) * 128, :], first=(hh == 0)))
        for h in range(8):
            heads.append(dict(kind="m", kv=h, kd=96, scale=96.0 ** -0.5, q=QmT[h * 96:(h + 1) * 96, :],
                              gt=GT[1024 + h * 128:1024 + (h + 1) * 128, :], dst=GOT[1024 + h * 128:1024 + (h + 1) * 128, :], first=True))
        steps = []
        for hi, hd in enumerate(heads):
            for j in range(NT):
                for kp in range(NKP):
                    steps.append((hi, j, kp))
        state = {"kv": None}

        def load_kv(hd):
            i = rot("kv", 2)
            if hd["kind"] == "g":
                g = hd["kv"]
                nsp = max(1, TP // 2048)
                for a in range(nsp):
                    sl = slice(a * (TP // nsp), (a + 1) * (TP // nsp))
                    ld(Kt[i][:, sl], KgT[g * 128:(g + 1) * 128, sl], [("K", i)])
                for a in range(nsp):
                    sl = slice(a * (NKT // nsp), (a + 1) * (NKT // nsp))
                    ld(Vt[i][:, sl, :], Vg[g, :, sl, :], [("V", i)])
            else:
                h = hd["kv"]
                nsp = max(1, TP // 2048)
                for a in range(nsp):
                    sl = slice(a * (TP // nsp), (a + 1) * (TP // nsp))
                    ld(Kt[i][0:64, sl], KnT[h * 64:(h + 1) * 64, sl], [("K", i)])
                    ld(Kt[i][64:96, sl], KrT[:, sl], [("K", i)])
                for a in range(nsp):
                    sl = slice(a * (NKT // nsp), (a + 1) * (NKT // nsp))
                    ld(Vt[i][:, sl, :], Vm[h, :, sl, :], [("V", i)])
            return i

        cur = {}

        def emit_qk(step):
            hi, j, kp = step
            hd = heads[hi]
            if j == 0 and kp == 0 and hd["first"]:
                if state.get("pre") is not None:
                    state["kv"] = state["pre"]
                    state["pre"] = None
                else:
                    state["kv"] = load_kv(hd)
            if j == NT - 1 and kp == 0 and hi + 1 < len(heads) and heads[hi + 1]["first"] and NT > 1:
                state["pre"] = load_kv(heads[hi + 1])
            kvi = state["kv"]
            kd = hd["kd"]
            if kp == 0:
                qi = rot("Q", 2)
                ld(Qt[qi][0:kd, :], hd["q"][:, j * 512:(j + 1) * 512], [("Q", qi)])
                ld(Gt[qi][:], hd["gt"][:, j * 512:(j + 1) * 512], [("G", qi)])
                cur[(hi, j)] = dict(qi=qi, kvi=kvi, ob=rot("O", 2))
            c = cur[(hi, j)]
            sbuf = rot("S", 3)
            c[("s", kp)] = sbuf
            for t in range(2):
                kt = kp * 2 + t
                mm(bank(sbuf * 2 + t), Kt[c["kvi"]][0:kd, kt * 128:(kt + 1) * 128], Qt[c["qi"]][0:kd, :], True, True,
                   [("K", c["kvi"]), ("Q", c["qi"])], [("S", sbuf)])

        def emit_pv(step):
            hi, j, kp = step
            hd = heads[hi]
            c = cur[(hi, j)]
            sbuf = c[("s", kp)]
            pi = rot("P", 3)
            act(Pb[pi][:], bank(sbuf * 2, 2).rearrange("p (k n) -> p k n", k=2), AF.Exp, [("S", sbuf), "const"], [("P", pi)],
                bias=kbias[:, kp:kp + 1], scale=hd["scale"])
            o = c["ob"]
            for t in range(2):
                kt = kp * 2 + t
                first = (kp == 0 and t == 0)
                last = (kp == NKP - 1 and t == 1)
                mm(bank(6), Vt[c["kvi"]][:, kt, :], Pb[pi][:, t, :], first, last, [("V", c["kvi"]), ("P", pi)], [("O", 0)])
            a = kp % 2

            def issue_sum(pend):
                b2_, first_, last_ = pend
                mm(bank(7), onesb[:], s2[b2_][:], first_, last_, [("s2", b2_), "const"], [("SUM", 0)])

            if a == 0 and c.get("pend") is not None:
                issue_sum(c["pend"])
                c["pend"] = None
            tt("dve", s1[a][:], Pb[pi][:, 0, :], Pb[pi][:, 1, :], ALU.add, [("P", pi)], [("s1", a)])
            if a == 1:
                b2 = rot("s2", 2)
                tt("dve", s2[b2][:], s1[0][:], s1[1][:], ALU.add, [("s1", 0), ("s1", 1)], [("s2", b2)])
                pend = (b2, kp == 1, kp == NKP - 1)
                if kp == NKP - 1:
                    issue_sum(pend)
                else:
                    c["pend"] = pend
            if kp == NKP - 1:
                recip(rs[o][:], bank(7), [("SUM", 0)], [("rs", o)])
                tt("dve", of[o][:], bank(6), rs[o][:], ALU.mult, [("O", 0), ("rs", o)], [("of", o)])
                tt("pool", ob[o][:], of[o][:], Gt[c["qi"]][:], ALU.mult, [("of", o), ("G", c["qi"])], [("ob", o)])
                st(hd["dst"][:, j * 512:(j + 1) * 512], ob[o][:], [("ob", o)])
                del cur[(hi, j)]

        emit_qk(steps[0])
        emit_qk(steps[1])
        for si in range(len(steps)):
            if si + 2 < len(steps):
                emit_qk(steps[si + 2])
            emit_pv(steps[si])
        P.barrier()

    def phase_C(l, xsrc, last):
        sb.reset(pbase)
        w_out_b = sb.alloc([128, 8, D], BF16)
        g_ffn = sb.alloc([128, D], F32)
        w_r = sb.alloc([128, 8, 72], F32)
        b_r = sb.alloc([128, 72], F32)
        mC = sb.mark()
        stg = [sb.alloc([128, 2048], F32) for _ in range(2)]
        wv = w_out_d[l].rearrange("(c p) n -> p c n", p=128)
        for c in range(8):
            load_cast(lambda c0, w, c=c: w_out_b[:, c, c0:c0 + w], lambda c0, w, c=c: wv[:, c, c0:c0 + w], D, stg, "stg", "w_out")
        ld(g_ffn[:], norm_ffn_d[l].partition_broadcast(128), ["g_ffn"])
        ld(w_r[:], w_r_d[l].rearrange("(c p) n -> p c n", p=128), ["w_r"])
        ld(b_r[:], b_r_d[l].partition_broadcast(128), ["b_r"])
        P.add("pool", lambda e: e.memset(basec[:], 0.0), writes=["basec"])
        P.barrier()
        sb.reset(mC)
        got = [sb.alloc([128, 24, 512], BF16) for _ in range(2)]
        xt = [sb.alloc([128, D], F32) for _ in range(2)]
        xn = [sb.alloc([128, D], F32) for _ in range(2)]
        h2s = [sb.alloc([128, D], F32) for _ in range(2)]
        h2b = [sb.alloc([128, D], BF16) for _ in range(2)]
        pendc = []

        def deferc(fn):
            if pendc:
                pendc.pop(0)()
            pendc.append(fn)
        h2T = sb.alloc([128, 8, 128], F32)
        junk = sb.alloc([128, D], BF16)
        msq = sb.alloc([128, 1], F32)
        rstd = sb.alloc([128, 1], F32)
        lg = sb.alloc([128, 72], F32)
        sm = {k: sb.alloc([128, 1], F32) for k in ("gmax", "ngmax", "gsum", "m1", "m2", "d21", "e21", "den", "d1", "d2")}
        ge = sb.alloc([128, 8], F32)
        ohg = sb.alloc([128, 8], F32)
        pen = sb.alloc([128, 8], F32)
        elm = sb.alloc([128, 64], F32)
        elm2 = sb.alloc([128, 64], F32)
        oh1 = sb.alloc([128, 64], F32)
        oh2 = sb.alloc([128, 64], F32)
        Asum = sb.alloc([128, 64], F32)
        Ab = sb.alloc([128, 64], BF16)
        rank = sb.alloc([128, 64], F32)
        slot = sb.alloc([128, 64], F32)
        ovf = sb.alloc([128, 64], F32)
        tmp = sb.alloc([128, 64], F32)
        BIG = 1.0e4
        BIGIDX = 1.0e6
        GOTv = GOT.rearrange("(bc p) t -> p bc t", p=128)
        for j in range(NT):
            gi = rot("got", 2)
            for b3 in range(3):
                ld(got[gi][:, b3 * 8:(b3 + 1) * 8, :], GOTv[:, b3 * 8:(b3 + 1) * 8, j * 512:(j + 1) * 512], [("got", gi)])
            for s in range(4):
                tile = j * 4 + s
                r0 = tile * 128
                xi = rot("xt", 2)
                ld(xt[xi][:], xsrc[r0:r0 + 128, :], [("xt", xi)])
                pb = rot("po", 2) * 2
                for n in range(2):
                    for bc in range(24):
                        mm(bank(pb + n), got[gi][:, bc, s * 128:(s + 1) * 128], w_out_b[:, bc % 8, n * 512:(n + 1) * 512], bc == 0, bc == 23,
                           [("got", gi), "w_out"], [("po", pb)])
                tt("dve", xn[xi][:], bank(pb, 2), xt[xi][:], ALU.add, [("po", pb), ("xt", xi)], [("xn", xi)])
                st(xmid[r0:r0 + 128, :], xn[xi][:], [("xn", xi)])
                act(junk[:], xn[xi][:], AF.Square, [("xn", xi)], ["junk", "msq"], scale=1.0 / 32.0, accum_out=msq[:])
                act(rstd[:], msq[:], AF.Sqrt, ["msq", "const"], ["rstd"], bias=epst[:, 0:1], scale=1.0)
                recip(rstd[:], rstd[:], ["rstd"], ["rstd"])
                hq = tile % 2
                h2 = h2s[hq]
                stt("dve", h2[:], xn[xi][:], rstd[:, 0:1], g_ffn[:], ALU.mult, ALU.mult, [("xn", xi), "rstd", "g_ffn"], [("h2", hq)])
                hi_ = rot("h2b", 2)
                cp("act", h2b[hi_][:], h2[:], [("h2", hq)], [("h2b", hi_)])

                def stage2(tile=tile, hi_=hi_, hq=hq, h2=h2):
                    for c in range(8):
                        tr(bank(4, 2)[:, c * 128:(c + 1) * 128], h2[:, c * 128:(c + 1) * 128], identf[:], [("h2", hq), "const"], [("ps", 4)])
                    cp("dve", h2T[:], bank(4, 2).rearrange("p (c n) -> p c n", c=8), [("ps", 4)], ["h2T"])
                    for c in range(8):
                        mm(bank(6)[:, 0:72], h2T[:, c, :], w_r[:, c, :], c == 0, c == 7, ["h2T", "w_r"], [("ps", 6)])
                    tt("dve", lg[:], bank(6)[:, 0:72], b_r[:], ALU.add, [("ps", 6), "b_r"], ["lg"])
                    rmax(sm["gmax"][:], lg[:, 0:8], ["lg"], ["gmax"])
                    ts("dve", sm["ngmax"][:], sm["gmax"][:], -1.0, None, ALU.mult, None, ["gmax"], ["ngmax"])
                    act(ge[:], lg[:, 0:8], AF.Exp, ["lg", "ngmax"], ["ge", "gsum"], bias=sm["ngmax"][:, 0:1], scale=1.0, accum_out=sm["gsum"][:])
                    ts("dve", ohg[:], lg[:, 0:8], sm["gmax"][:, 0:1], None, ALU.is_ge, None, ["lg", "gmax"], ["ohg"])
                    ts("dve", pen[:], ohg[:], -1.0, BIG, ALU.add, ALU.mult, ["ohg"], ["pen"])
                    tt("dve", elm[:].rearrange("p (g e) -> p g e", g=8), lg[:, 8:72].rearrange("p (g e) -> p g e", g=8),
                       pen[:].unsqueeze(2).to_broadcast([128, 8, 8]), ALU.add, ["lg", "pen"], ["elm"])
                    rmax(sm["m1"][:], elm[:], ["elm"], ["m1"])
                    ts("dve", oh1[:], elm[:], sm["m1"][:, 0:1], None, ALU.is_ge, None, ["elm", "m1"], ["oh1"])
                    stt("dve", elm2[:], oh1[:], -BIG, elm[:], ALU.mult, ALU.add, ["oh1", "elm"], ["elm2"])
                    rmax(sm["m2"][:], elm2[:], ["elm2"], ["m2"])
                    ts("dve", oh2[:], elm2[:], sm["m2"][:, 0:1], None, ALU.is_ge, None, ["elm2", "m2"], ["oh2"])
                    tt("dve", sm["d21"][:], sm["m2"][:], sm["m1"][:], ALU.subtract, ["m1", "m2"], ["d21"])
                    act(sm["e21"][:], sm["d21"][:], AF.Exp, ["d21"], ["e21"])
                    ts("dve", sm["den"][:], sm["e21"][:], 1.0, sm["gsum"][:, 0:1], ALU.add, ALU.mult, ["e21", "gsum"], ["den"])
                    recip(wts[:, tile, 0:1], sm["den"][:], ["den"], [("wts", tile)])
                    tt("dve", wts[:, tile, 1:2], wts[:, tile, 0:1], sm["e21"][:], ALU.mult, [("wts", tile), "e21"], [("wts", tile)])
                    tt("dve", Asum[:], oh1[:], oh2[:], ALU.add, ["oh1", "oh2"], ["Asum"])
                    ts("dve", Ab[:], Asum[:], tvalid[:, tile:tile + 1], None, ALU.mult, None, ["Asum", "const"], ["Ab"])
                    mm(bank(7)[:, 0:64], ustr[:], Ab[:], True, True, ["Ab", "const"], [("ps", 7)])
                    mm(bank(7)[:, 64:128], onesb[:], Ab[:], True, True, ["Ab", "const"], [("ps", 7)])
                    tt("dve", rank[:], bank(7)[:, 0:64], basec[:], ALU.add, [("ps", 7), "basec"], ["rank"])
                    tt("dve", basec[:], bank(7)[:, 64:128], basec[:], ALU.add, [("ps", 7), "basec"], ["basec"])
                    ts("dve", ovf[:], rank[:], float(CAP), BIGIDX, ALU.is_ge, ALU.mult, ["rank"], ["ovf"])
                    tt("dve", slot[:], rank[:], eoff[:], ALU.add, ["rank", "const"], ["slot"])
                    tt("dve", slot[:], slot[:], ovf[:], ALU.add, ["slot", "ovf"], ["slot"])
                    for k, oh, dk in ((0, oh1, "d1"), (1, oh2, "d2")):
                        tt("dve", tmp[:], oh[:], slot[:], ALU.mult, ["oh1", "oh2", "slot"], ["tmp"])
                        rsum(sm[dk][:], tmp[:], ["tmp"], [dk])
                        ts("dve", sm[dk][:], sm[dk][:], invbig[:, tile:tile + 1], None, ALU.add, None, [dk, "const"], [dk])
                        cp("dve", dest_i[:, tile, k:k + 1], sm[dk][:], [dk], [("dest", tile)])
                        P.add("pool", lambda e, k=k, tile=tile, hi_=hi_: e.indirect_dma_start(
                            out=xs, out_offset=bass.IndirectOffsetOnAxis(ap=dest_i[:, tile, k:k + 1], axis=0),
                            in_=h2b[hi_][:], in_offset=None, bounds_check=regs["bc"], oob_is_err=False),
                            reads=[("h2b", hi_), ("dest", tile)], writes=[], grp="ind")

                deferc(stage2)
        while pendc:
            pendc.pop(0)()
        P.barrier()
        chk("C1")
        sb.reset(pbase)
        CR = CAP
        wgb = [sb.alloc([128, 8, 512], BF16) for _ in range(3)]
        wdb = [sb.alloc([128, 2, D], BF16) for _ in range(3)]
        xsb = [sb.alloc([128, CB, D], BF16) for _ in range(3)]
        xbT = sb.alloc([128, 8, CR], BF16)
        sil = sb.alloc([128, 2, CR], F32)
        actT = sb.alloc([128, 2, CR], BF16)
        yo = [sb.alloc([128, D], F32) for _ in range(2)]
        def c2_loads(e_):
            wi = e_ % 3
            gv = w_gu_d[l, e_].rearrange("(c p) n -> p c n", p=128)
            for hlf in range(2):
                ld(wgb[wi][:, hlf * 4:(hlf + 1) * 4, :], gv[:, hlf * 4:(hlf + 1) * 4, :], [("wgb", wi)], eng="pool", grp="ldc")
            ld(wdb[wi][:], w_dn_d[l, e_].rearrange("(c p) n -> p c n", p=128), [("wdb", wi)], eng="pool", grp="ldc")
            ld(xsb[wi][:], xs[e_ * CAP:(e_ + 1) * CAP, :].rearrange("(b p) d -> p b d", p=128), [("xsb", wi)])

        c2_loads(0)
        c2_loads(1)
        for e_ in range(64):
            wi = e_ % 3
            if e_ + 2 < 64:
                c2_loads(e_ + 2)
            for b in range(CB):
                tb = rot("tb", 2)
                pT = bank(tb).bitcast(BF16)
                for c in range(8):
                    tr(pT[:, c * 128:(c + 1) * 128], xsb[wi][:, b, c * 128:(c + 1) * 128], identb[:], [("xsb", wi), "const"], [("ps", tb)])
                cp("dve", xbT[:, :, b * 128:(b + 1) * 128], pT.rearrange("p (c n) -> p c n", c=8), [("ps", tb)], ["xbT"])
            for ch in range(4):
                for c in range(8):
                    mm(bank(2 + ch)[:, 0:CR], wgb[wi][:, c, ch * 128:(ch + 1) * 128], xbT[:, c, :], c == 0, c == 7, [("wgb", wi), "xbT"], [("ps", 2 + ch)])
            for fc in range(2):
                act(sil[:, fc, :], bank(2 + fc)[:, 0:CR], AF.Silu, [("ps", 2 + fc)], [("sil", fc)])
                tt("dve", actT[:, fc, :], sil[:, fc, :], bank(4 + fc)[:, 0:CR], ALU.mult, [("sil", fc), ("ps", 4 + fc)], ["actT"])
            for b in range(CB):
                db = 6 if b % 2 == 0 else 0
                dkeys = [("ps", db), ("ps", db + 1)]
                for n in range(2):
                    for fc in range(2):
                        mm(bank(db + n), actT[:, fc, b * 128:(b + 1) * 128], wdb[wi][:, fc, n * 512:(n + 1) * 512], fc == 0, fc == 1, ["actT", ("wdb", wi)], dkeys)
                yi = rot("yo", 2)
                cp("act" if b % 2 == 0 else "dve", yo[yi][:], bank(db, 2), dkeys, [("yo", yi)])
                r0 = e_ * CAP + b * 128
                st_sp(ys[r0:r0 + 128, :], yo[yi][:], [("yo", yi)])
        P.barrier()
        chk("C2")
        sb.reset(pbase)
        y0 = [sb.alloc([128, D], F32) for _ in range(4)]
        y1 = [sb.alloc([128, D], F32) for _ in range(4)]
        xn = [sb.alloc([128, D], F32) for _ in range(4)]
        acc = [sb.alloc([128, D], F32) for _ in range(4)]
        outt = [sb.alloc([128, D], F32) for _ in range(4)]
        g_fin = sb.alloc([128, D], F32)
        junk = sb.alloc([128, D], BF16)
        msq = sb.alloc([128, 1], F32)
        rstd = sb.alloc([128, 1], F32)
        ld(g_fin[:], norm_final_d.partition_broadcast(128), ["g_fin"])
        for i in range(4):
            P.add("pool", lambda e, i=i: e.memset(y0[i][:], 0.0), writes=[("y0", i)])
            P.add("pool", lambda e, i=i: e.memset(y1[i][:], 0.0), writes=[("y1", i)])
        for tile in range(NKT):
            r0 = tile * 128
            i = rot("c3", 4)
            ld(xn[i][:], xmid[r0:r0 + 128, :], [("xn", i)])
            for k, yy, yk in ((0, y0, "y0"), (1, y1, "y1")):
                P.add("pool", lambda e, k=k, yy=yy, i=i, tile=tile: e.indirect_dma_start(
                    out=yy[i][:], out_offset=None, in_=ys,
                    in_offset=bass.IndirectOffsetOnAxis(ap=dest_i[:, tile, k:k + 1], axis=0),
                    bounds_check=regs["bc"], oob_is_err=False), reads=[], writes=[(yk, i)], grp="ind")
            stt("dve", acc[i][:], y0[i][:], wts[:, tile, 0:1], xn[i][:], ALU.mult, ALU.add, [("y0", i), ("xn", i)], [("acc", i)])
            if not last:
                stt("dve", outt[i][:], y1[i][:], wts[:, tile, 1:2], acc[i][:], ALU.mult, ALU.add, [("y1", i), ("acc", i)], [("outt", i)])
                st(xres[r0:r0 + 128, :], outt[i][:], [("outt", i)])
            else:
                stt("dve", acc[i][:], y1[i][:], wts[:, tile, 1:2], acc[i][:], ALU.mult, ALU.add, [("y1", i), ("acc", i)], [("acc", i)])
                act(junk[:], acc[i][:], AF.Square, [("acc", i)], ["junk", "msq"], scale=1.0 / 32.0, accum_out=msq[:])
                act(rstd[:], msq[:], AF.Sqrt, ["msq", "const"], ["rstd"], bias=epst[:, 0:1], scale=1.0)
                recip(rstd[:], rstd[:], ["rstd"], ["rstd"])
                stt("dve", outt[i][:], acc[i][:], rstd[:, 0:1], g_fin[:], ALU.mult, ALU.mult, [("acc", i), "rstd", "g_fin"], [("outt", i)])
                st(y_out[r0:r0 + 128, :], outt[i][:], [("outt", i)])
        P.barrier()

    xsrc = x_in
    try:
        for l in range(L):
            phase_A(l, xsrc)
            chk("A")
            phase_B(l)
            chk("B")
            phase_C(l, xsrc, last=(l == L - 1))
            xsrc = xres
    except _Stop:
        P.barrier()
    P.emit()
    return nc


def _rope_tables(TP, rot_dim):
    rows = TP // 64
    row_idx = np.repeat(np.arange(rows, dtype=np.float32), 64)
    col_idx = np.tile(np.arange(64, dtype=np.float32), rows)
    n_freq = rot_dim // 4
    inv_freq = (np.float32(10000.0) ** (-np.arange(n_freq, dtype=np.float32) / np.float32(n_freq))).astype(np.float32)
    ang = np.concatenate([row_idx[:, None] * inv_freq, col_idx[:, None] * inv_freq], axis=-1).astype(np.float32)
    return np.cos(ang).astype(np.float32), np.sin(ang).astype(np.float32)


def _rot_T(n, base, half):
    R = np.zeros((n, n), np.float32)
    for i in range(half):
        R[base + i, base + i + half] = -1.0
        R[base + i + half, base + i] = 1.0
    return np.ascontiguousarray(R.T)


def make_consts(TP, CAP):
    bf = ml_dtypes.bfloat16
    cg, sg = _rope_tables(TP, 128)
    cm, sm = _rope_tables(TP, 32)
    c = {}
    c["cgT"] = np.ascontiguousarray(np.concatenate([cg, cg], axis=1).T)
    c["sgT"] = np.ascontiguousarray(np.concatenate([sg, sg], axis=1).T)
    cmT = np.ones((96, TP), np.float32)
    smT = np.zeros((96, TP), np.float32)
    cmT[64:96] = np.concatenate([cm, cm], axis=1).T
    smT[64:96] = np.concatenate([sm, sm], axis=1).T
    c["cmT"] = cmT
    c["smT"] = smT
    c["identb"] = np.eye(128, dtype=np.float32).astype(bf)
    c["identf"] = np.eye(128, dtype=np.float32)
    c["onesb"] = np.ones((128, 128), np.float32).astype(bf)
    c["onesf"] = np.ones((128, 128), np.float32)
    c["ustr"] = np.triu(np.ones((128, 128), np.float32), 1).astype(bf)
    c["rgT"] = _rot_T(128, 0, 64)
    c["rmT"] = _rot_T(96, 64, 16)
    c["rkT"] = _rot_T(32, 0, 16)
    c["eoff"] = np.ascontiguousarray(np.broadcast_to((np.arange(64, dtype=np.float32) * CAP)[None, :], (128, 64)))
    return c


def core_inputs(x, mem, S, TP, shared):
    NKT = TP // 128
    NKP = NKT // 2
    xp = np.zeros((TP, D), np.float32)
    xp[:S] = x
    d = dict(shared)
    d["x"] = xp
    d["mem"] = np.ascontiguousarray(mem, dtype=np.float32)
    kb = np.where(np.arange(NKP) * 256 < S, 0.0, -30000.0).astype(np.float32)
    d["kbias"] = np.ascontiguousarray(np.broadcast_to(kb[None, :], (128, NKP)))
    tok = np.arange(NKT)[None, :] * 128 + np.arange(128)[:, None]
    valid = (tok < S).astype(np.float32)
    d["tvalid"] = valid
    d["invbig"] = ((1.0 - valid) * 1.0e6).astype(np.float32)
    return d


def shared_inputs(TP, CAP, w):
    s = make_consts(TP, CAP)
    f = lambda a: np.ascontiguousarray(np.asarray(a, dtype=np.float32))
    s["w_in"] = f(w["w_in"])
    s["w_qb"] = f(w["mla_w_qb"])
    kvb = f(w["mla_w_kvb"]).reshape(-1, 256, 8, 192)
    s["w_kvbk"] = np.ascontiguousarray(kvb[:, :, :, :64].reshape(-1, 256, 512))
    s["w_kvbv"] = np.ascontiguousarray(kvb[:, :, :, 64:].reshape(-1, 256, 1024))
    s["mem_w_kv"] = f(w["mem_w_kv"])
    s["w_out"] = f(w["w_out"])
    s["w_r"] = np.ascontiguousarray(np.concatenate([f(w["w_group"]), f(w["w_expert"])], axis=-1))
    s["b_r"] = np.ascontiguousarray(np.concatenate([f(w["b_group"]), f(w["b_expert"])], axis=-1))
    s["w_gu"] = f(w["w_gate_up"])
    s["w_dn"] = f(w["w_down"])
    for k in ("norm_mix", "norm_mem", "norm_ffn", "gqa_q_norm", "gqa_k_norm", "mla_q_a_norm", "mla_kv_a_norm", "norm_final"):
        s[k] = f(w[k])
    return s


def kernel(x_prompt, x_sample, mem_prompt, mem_sample, **w):
    TP = 8192
    depth = 2
    CAP = max(1, (4 * TP // 64 + 127) // 128) * 128
    x_prompt = np.asarray(x_prompt)
    x_sample = np.asarray(x_sample)
    mem_prompt = np.asarray(mem_prompt)
    mem_sample = np.asarray(mem_sample)
    shared = shared_inputs(TP, CAP, w)
    in_maps = []
    for b in range(4):
        in_maps.append(core_inputs(x_prompt[b], mem_prompt[b], 4096, TP, shared))
    for b in range(4):
        in_maps.append(core_inputs(x_sample[b], mem_sample[b], 8192, TP, shared))
    nc = build(TP, depth)
    res = run_bass_kernel_spmd(nc, in_maps, core_ids=list(range(8)))
    yp = np.stack([np.asarray(res.results[b]["y"])[:4096] for b in range(4)]).astype(np.float32)
    ysm = np.stack([np.asarray(res.results[4 + b]["y"]) for b in range(4)]).astype(np.float32)
    return (yp, ysm)
```
